# Optimizing a Trainium2 kernel written in Bass

```python
import math
import jax, jax.numpy as jnp
from jax import lax
import numpy as np

D_MODEL = 1024
BATCH = 16
SEQ = 2048
DEPTH = 4

GLA_HEADS = 4
GLA_DK = 32
GLA_DV = 64
GLA_RANK = 16
GLA_GATE_TEMP = 16.0
DIFF_HEADS = 4
DIFF_HD = 64
DIFF_DV = 2 * DIFF_HD
HGRN_HEADS = 4
HGRN_DK = 64
HGRN_DV = 64
CHUNK = 16
Q_BLOCK = 128
ROPE_THETA = 10000.0
EPS = 1e-6
F_FLOOR = 1e-30

GLA_W = GLA_HEADS * GLA_DV
DIFF_W = DIFF_HEADS * DIFF_DV
HGRN_W = HGRN_HEADS * HGRN_DV
MIX_W = GLA_W + DIFF_W + HGRN_W

IN_SIZES = (
    GLA_HEADS * GLA_DK, GLA_HEADS * GLA_DK, GLA_W, GLA_W, GLA_RANK, GLA_RANK,
    DIFF_HEADS * 2 * DIFF_HD, DIFF_HEADS * 2 * DIFF_HD, DIFF_W, DIFF_W,
    HGRN_HEADS * HGRN_DK, HGRN_HEADS * HGRN_DK, HGRN_HEADS * HGRN_DK, HGRN_W, HGRN_W,
)
IN_W = sum(IN_SIZES)

kernel_name = "hybrid_gla_diffattn_hgrn2_bidir_encoder"


def _offsets():
    offs, run = [], 0
    for s in IN_SIZES[:-1]:
        run += s
        offs.append(run)
    return offs


def rms_norm(x, w):
    xf = x.astype(jnp.float32)
    xf = xf * lax.rsqrt(jnp.mean(xf * xf, axis=-1, keepdims=True) + EPS)
    return xf.astype(x.dtype) * w


def split_heads(t, n_heads):
    b, l, _ = t.shape
    return t.reshape(b, l, n_heads, -1).transpose(0, 2, 1, 3)


def merge_heads(t):
    b, h, l, d = t.shape
    return t.transpose(0, 2, 1, 3).reshape(b, l, h * d)


def rope(x, pos):
    d = x.shape[-1]
    inv_freq = ROPE_THETA ** (-jnp.arange(0, d, 2, dtype=jnp.float32) / d)
    ang = pos.astype(jnp.float32)[:, None] * inv_freq[None, :]
    cos, sin = jnp.cos(ang).astype(x.dtype), jnp.sin(ang).astype(x.dtype)
    x1, x2 = x[..., : d // 2], x[..., d // 2:]
    return jnp.concatenate([x1 * cos - x2 * sin, x2 * cos + x1 * sin], axis=-1)


def chunked_gated_scan(q, k, v, log_g):
    b_, h_, l_, dk = q.shape
    dv = v.shape[-1]
    n = l_ // CHUNK
    r = lambda t: t.reshape(b_, h_, n, CHUNK, t.shape[-1])
    q, k, v, lg = r(q), r(k), r(v), r(log_g)
    bcum = jnp.cumsum(lg.astype(jnp.float32), axis=3)
    mask = jnp.tril(jnp.ones((CHUNK, CHUNK), dtype=bool))[:, :, None]
    diff = bcum[:, :, :, :, None, :] - bcum[:, :, :, None, :, :]
    decay = jnp.where(mask, jnp.exp(jnp.where(mask, diff, 0.0)), 0.0)
    attn = jnp.einsum('bhntk,bhnsk,bhntsk->bhnts', q, k, decay.astype(q.dtype))
    o_intra = jnp.einsum('bhnts,bhnsv->bhntv', attn, v)
    b_last = bcum[:, :, :, -1:, :]
    u = jnp.einsum('bhnsk,bhnsv->bhnkv', k * jnp.exp(b_last - bcum).astype(k.dtype), v)
    g_chunk = jnp.exp(b_last[:, :, :, 0, :]).astype(u.dtype)

    def step(state, inp):
        g_n, u_n = inp
        return g_n[..., None] * state + u_n, state

    s0 = jnp.zeros((b_, h_, dk, dv), u.dtype)
    _, s_prev = lax.scan(step, s0, (jnp.moveaxis(g_chunk, 2, 0), jnp.moveaxis(u, 2, 0)))
    s_prev = jnp.moveaxis(s_prev, 0, 2)
    o_inter = jnp.einsum('bhntk,bhnkv->bhntv', q * jnp.exp(bcum).astype(q.dtype), s_prev)
    return (o_intra + o_inter).reshape(b_, h_, l_, dv).astype(v.dtype)


def bidir_scan(q, k_f, k_b, v, lg_f, lg_b):
    flip = lambda t: jnp.flip(t, axis=2)
    o_f = chunked_gated_scan(q, k_f, v, lg_f)
    o_b = flip(chunked_gated_scan(flip(q), flip(k_b), flip(v), flip(lg_b)))
    return o_f + o_b


def gla_branch(q, k, v, g, a_f, a_b, wa2_f, ba_f, wa2_b, ba_b, norm_w):
    q = split_heads(q, GLA_HEADS) * (GLA_DK ** -0.5)
    k = split_heads(k, GLA_HEADS)
    v = split_heads(v, GLA_HEADS)
    lg_f = split_heads(jax.nn.log_sigmoid((a_f @ wa2_f + ba_f).astype(jnp.float32)) / GLA_GATE_TEMP, GLA_HEADS)
    lg_b = split_heads(jax.nn.log_sigmoid((a_b @ wa2_b + ba_b).astype(jnp.float32)) / GLA_GATE_TEMP, GLA_HEADS)
    o = bidir_scan(q, k, k, v, lg_f, lg_b)
    o = rms_norm(o, norm_w)
    return merge_heads(o) * jax.nn.silu(g)


def diff_branch(q, k, v, g, lq1, lk1, lq2, lk2, norm_w, lambda_init, pos):
    b_, l_, _ = q.shape
    q = q.reshape(b_, l_, DIFF_HEADS, 2, DIFF_HD).transpose(3, 0, 2, 1, 4)
    k = k.reshape(b_, l_, DIFF_HEADS, 2, DIFF_HD).transpose(3, 0, 2, 1, 4)
    q = rope(q, pos) * (DIFF_HD ** -0.5)
    k = rope(k, pos)
    v = split_heads(v, DIFF_HEADS)
    lam = (jnp.exp(jnp.sum(lq1 * lk1).astype(jnp.float32))
           - jnp.exp(jnp.sum(lq2 * lk2).astype(jnp.float32)) + lambda_init)
    nb = l_ // Q_BLOCK
    qb = jnp.moveaxis(q.reshape(2, b_, DIFF_HEADS, nb, Q_BLOCK, DIFF_HD), 3, 0)

    def block(qblk):
        s = jnp.einsum('cbhqd,cbhkd->cbhqk', qblk, k).astype(jnp.float32)
        p = jax.nn.softmax(s, axis=-1)
        w = p[0] - lam * p[1]
        return jnp.einsum('bhqk,bhkv->bhqv', w.astype(v.dtype), v)

    o = lax.map(block, qb)
    o = jnp.moveaxis(o, 0, 2).reshape(b_, DIFF_HEADS, l_, DIFF_DV)
    o = rms_norm(o, norm_w) * (1.0 - lambda_init)
    return merge_heads(o) * jax.nn.silu(g)


def hgrn_branch(q, z_f, z_b, i, g, lb, norm_w):
    lb32 = lb.astype(jnp.float32)

    def gates(z):
        z32 = z.astype(jnp.float32)
        one_minus_f = (1.0 - lb32) * jax.nn.sigmoid(-z32)
        f = lb32 + (1.0 - lb32) * jax.nn.sigmoid(z32)
        log_f = jnp.log(jnp.maximum(f, F_FLOOR))
        return split_heads(log_f, HGRN_HEADS), split_heads(one_minus_f.astype(z.dtype), HGRN_HEADS)

    lg_f, k_f = gates(z_f)
    lg_b, k_b = gates(z_b)
    o = bidir_scan(split_heads(q, HGRN_HEADS), k_f, k_b, split_heads(i, HGRN_HEADS), lg_f, lg_b)
    o = rms_norm(o, norm_w)
    return merge_heads(o) * jax.nn.silu(g)


def setup_inputs(seed: int = 0) -> dict:
    key = jax.random.key(seed)
    ks = jax.random.split(key, 20)
    nrm = lambda k, shape, s: jax.random.normal(k, shape, jnp.float32) * s
    return {
        "x": nrm(ks[0], (BATCH, SEQ, D_MODEL), 1.0),
        "norm_pre": 1.0 + nrm(ks[1], (DEPTH, D_MODEL), 0.05),
        "norm_post": 1.0 + nrm(ks[2], (DEPTH, D_MODEL), 0.05),
        "w_in": nrm(ks[3], (DEPTH, D_MODEL, IN_W), D_MODEL ** -0.5),
        "w_out": nrm(ks[4], (DEPTH, MIX_W, D_MODEL), MIX_W ** -0.5),
        "gla_wa2_fwd": nrm(ks[5], (DEPTH, GLA_RANK, GLA_HEADS * GLA_DK), GLA_RANK ** -0.5),
        "gla_ba_fwd": nrm(ks[6], (DEPTH, GLA_HEADS * GLA_DK), 0.1),
        "gla_wa2_bwd": nrm(ks[7], (DEPTH, GLA_RANK, GLA_HEADS * GLA_DK), GLA_RANK ** -0.5),
        "gla_ba_bwd": nrm(ks[8], (DEPTH, GLA_HEADS * GLA_DK), 0.1),
        "gla_norm": 1.0 + nrm(ks[9], (DEPTH, GLA_DV), 0.05),
        "diff_lq1": nrm(ks[10], (DEPTH, DIFF_HD), 0.1),
        "diff_lk1": nrm(ks[11], (DEPTH, DIFF_HD), 0.1),
        "diff_lq2": nrm(ks[12], (DEPTH, DIFF_HD), 0.1),
        "diff_lk2": nrm(ks[13], (DEPTH, DIFF_HD), 0.1),
        "diff_norm": 1.0 + nrm(ks[14], (DEPTH, DIFF_DV), 0.05),
        "hgrn_lb_logits": nrm(ks[15], (DEPTH, HGRN_HEADS * HGRN_DK), 0.1),
        "hgrn_norm": 1.0 + nrm(ks[16], (DEPTH, HGRN_DV), 0.05),
    }


def reference(x, norm_pre, norm_post, w_in, w_out, gla_wa2_fwd, gla_ba_fwd, gla_wa2_bwd, gla_ba_bwd,
              gla_norm, diff_lq1, diff_lk1, diff_lq2, diff_lk2, diff_norm, hgrn_lb_logits, hgrn_norm):
    pos = jnp.arange(x.shape[1], dtype=jnp.int32)
    p_lb = jax.nn.softmax(hgrn_lb_logits.astype(jnp.float32), axis=0)
    lb_all = jnp.cumsum(p_lb, axis=0) - p_lb[0:1]
    offs = _offsets()
    for layer in range(DEPTH):
        lambda_init = 0.8 - 0.6 * math.exp(-0.3 * layer)
        h = rms_norm(x, norm_pre[layer])
        proj = h @ w_in[layer]
        (gq, gk, gv, gg, gaf, gab, dq, dk, dv, dg, hq, hzf, hzb, hi, hg) = jnp.split(proj, offs, axis=-1)
        y_a = gla_branch(gq, gk, gv, gg, gaf, gab, gla_wa2_fwd[layer], gla_ba_fwd[layer],
                         gla_wa2_bwd[layer], gla_ba_bwd[layer], gla_norm[layer])
        y_b = diff_branch(dq, dk, dv, dg, diff_lq1[layer], diff_lk1[layer], diff_lq2[layer], diff_lk2[layer],
                          diff_norm[layer], lambda_init, pos)
        y_c = hgrn_branch(hq, hzf, hzb, hi, hg, lb_all[layer], hgrn_norm[layer])
        y = jnp.concatenate([y_a, y_b, y_c], axis=-1) @ w_out[layer]
        x = x + rms_norm(y, norm_post[layer])
    return x
```

```python
import math
import bisect
import types
from contextlib import ExitStack

import numpy as np
import concourse.bass as bass
import concourse.mybir as mybir
from concourse.bass_utils import run_bass_kernel_spmd

F32 = mybir.dt.float32
BF16 = mybir.dt.bfloat16
AF = mybir.ActivationFunctionType
ALU = mybir.AluOpType
AX = mybir.AxisListType

L = 2048
D = 1024
NB = 16
NSEG = 4
IN_W = 4128
EPS = 1e-6
N_CORES = 8

ENGS = ("pe", "act", "dve", "pool", "sp")
SEM_LIMIT = 20000


class Op:
    __slots__ = ("eng", "fn", "reads", "writes", "gi", "dma", "deps", "bdeps", "sig", "sigidx", "waits", "dmaval")

    def __init__(self, eng, fn, reads, writes, dma):
        self.eng = eng
        self.fn = fn
        self.reads = reads
        self.writes = writes
        self.dma = dma
        self.deps = set()
        self.sig = False
        self.sigidx = None
        self.waits = []
        self.dmaval = None


def _freeze(fn):
    if fn.__closure__ is None:
        return fn
    cells = []
    for c in fn.__closure__:
        try:
            cells.append(types.CellType(c.cell_contents))
        except ValueError:
            cells.append(c)
    return types.FunctionType(fn.__code__, fn.__globals__, fn.__name__, fn.__defaults__, tuple(cells))


class Sched:
    def __init__(self, nc):
        self.nc = nc
        self.ops = []
        self.last_writer = {}
        self.readers = {}
        self.barrier_ops = []

    def add(self, eng, fn, reads=(), writes=(), dma=None):
        op = Op(eng, _freeze(fn), tuple(reads), tuple(writes), dma)
        op.gi = len(self.ops)
        deps = op.deps
        for r in op.reads:
            w = self.last_writer.get(r)
            if w is not None:
                deps.add(w)
        for r in op.writes:
            w = self.last_writer.get(r)
            if w is not None and not (dma is not None and w.dma == dma):
                deps.add(w)
            for rd in self.readers.get(r, ()):
                deps.add(rd)
        op.bdeps = self.barrier_ops
        deps.discard(op)
        for r in op.writes:
            self.last_writer[r] = op
            self.readers[r] = []
        for r in op.reads:
            self.readers.setdefault(r, []).append(op)
        self.ops.append(op)
        return op

    def barrier(self):
        last = {}
        for op in self.ops:
            key = ("dma", op.dma) if op.dma is not None else ("eng", op.eng)
            last[key] = op
        self.barrier_ops = list(last.values())
        self.last_writer = {}
        self.readers = {}

    def emit(self, stack):
        nc = self.nc
        ops = self.ops

        def unsynced(d, op):
            return d.dma is None and op.dma is None and d.eng == "pe" and op.eng == "pe"

        for op in ops:
            for d in op.deps:
                if not unsynced(d, op):
                    d.sig = True
            for d in op.bdeps:
                if d is not op and d.dma is None:
                    d.sig = True
        sigcount = {e: 0 for e in ENGS}
        dmacount = {}
        dma_hist = {}
        for op in ops:
            if op.dma is not None:
                dmacount[op.dma] = dmacount.get(op.dma, 0) + 16
                op.dmaval = dmacount[op.dma]
                dma_hist.setdefault(op.dma, []).append((op.gi, op.dmaval))
            elif op.sig:
                sigcount[op.eng] += 1
                op.sigidx = sigcount[op.eng]
        sems = {e: [stack.enter_context(nc.semaphore(f"s_{e}_{i}"))
                    for i in range(sigcount[e] // SEM_LIMIT + 1)] for e in ENGS}
        dsems = {}
        for k, v in dmacount.items():
            assert v < 30000, (k, v)
            dsems[k] = stack.enter_context(nc.semaphore(f"d_{k}"))

        def semval(eng, idx):
            return sems[eng][(idx - 1) // SEM_LIMIT], (idx - 1) % SEM_LIMIT + 1

        waited = {e: {s: 0 for s in ENGS} for e in ENGS}
        dwaited = {e: {} for e in ENGS}
        for op in ops:
            need = {}
            dneed = {}
            for d in op.deps:
                if d.dma is not None:
                    hist = dma_hist[d.dma]
                    pos = bisect.bisect_left(hist, (op.gi, 0)) - 1
                    val = hist[pos][1]
                    assert val >= d.dmaval
                    dneed[d.dma] = max(dneed.get(d.dma, 0), val)
                elif not unsynced(d, op):
                    need[d.eng] = max(need.get(d.eng, 0), d.sigidx)
            for d in op.bdeps:
                if d is op:
                    continue
                if d.dma is not None:
                    dneed[d.dma] = max(dneed.get(d.dma, 0), d.dmaval)
                else:
                    need[d.eng] = max(need.get(d.eng, 0), d.sigidx)
            w = []
            for s, idx in need.items():
                if idx > waited[op.eng][s]:
                    waited[op.eng][s] = idx
                    w.append(semval(s, idx))
            for k, val in dneed.items():
                if val > dwaited[op.eng].get(k, 0):
                    dwaited[op.eng][k] = val
                    w.append((dsems[k], val))
            op.waits = w

        block = stack.enter_context(nc.Block())
        per_eng = {e: [op for op in ops if op.eng == e] for e in ENGS}

        def run(engname, engine):
            lst = per_eng[engname]
            for op in lst:
                for (sem, val) in op.waits:
                    engine.wait_ge(sem, val)
                ins = op.fn(engine)
                if op.dma is not None:
                    ins.then_inc(dsems[op.dma], 16)
                elif op.sig:
                    ins.then_inc(semval(engname, op.sigidx)[0], 1)
            done = {}
            for op in lst:
                if op.dma is not None:
                    done[op.dma] = dmacount[op.dma]
            for k, val in done.items():
                engine.wait_ge(dsems[k], val)

        @block.tensor
        def _(e):
            run("pe", e)

        @block.scalar
        def _(e):
            run("act", e)

        @block.vector
        def _(e):
            run("dve", e)

        @block.gpsimd
        def _(e):
            run("pool", e)

        @block.sync
        def _(e):
            run("sp", e)


PARAM_SPECS = [
    ("norm_pre", [4, 1024]), ("norm_post", [4, 1024]), ("wpk", [4, 11, 128, 4096]),
    ("gla_wa2_fwd", [4, 16, 128]), ("gla_ba_fwd", [4, 128]), ("gla_wa2_bwd", [4, 16, 128]), ("gla_ba_bwd", [4, 128]),
    ("gla_norm", [4, 64]), ("diff_lq1", [4, 64]), ("diff_lk1", [4, 64]), ("diff_lq2", [4, 64]), ("diff_lk2", [4, 64]),
    ("diff_norm", [4, 128]), ("hgrn_lb_logits", [4, 256]), ("hgrn_norm", [4, 64]),
]


DBG = {"tiles": 3, "stage": 99}


def build(n_layers=4, n_seq=2, dbg=False, stop=9):
    nc = bass.Bass("TRN2", target_bir_lowering=False)
    x_d = nc.dram_tensor("x", [n_seq, L, D], F32, kind="ExternalInput").ap()
    out_d = nc.dram_tensor("out", [n_seq, L, D], F32, kind="ExternalOutput").ap()
    P = {}
    for name, shp in PARAM_SPECS:
        P[name] = nc.dram_tensor(name, shp, F32, kind="ExternalInput").ap()
    if dbg:
        dbg_y = nc.dram_tensor("dbg_y", [128, 8, L], BF16, kind="ExternalOutput").ap()
        dbg_h = nc.dram_tensor("dbg_h", [128, 8, L], BF16, kind="ExternalOutput").ap()

    st = ExitStack()
    S = Sched(nc)

    def sb(name, shape, dt):
        return st.enter_context(nc.sbuf_tensor(name, shape, dt))

    x_sb = sb("x_sb", [128, NB, D], F32)
    hT = sb("hT", [128, 8, L], BF16)
    yT = sb("yT", [128, 8, L], BF16)
    wbuf = [sb(f"wbuf{i}", [128, 8, 512], BF16) for i in range(2)]
    gpost = sb("gpost", [128, D], F32)
    cos_t = sb("cos_t", [128, L], BF16)
    sin_t = sb("sin_t", [128, L], BF16)
    ident = sb("ident", [128, 128], BF16)
    maskF = sb("maskF", [128, 128], BF16)
    maskB = sb("maskB", [128, 128], BF16)
    bones = sb("bones", [128, 128], BF16)
    rmask = sb("rmask", [128, 512], BF16)
    gpreT = sb("gpreT", [128, 2, 8], F32)
    waf = sb("waf", [32, 128], BF16)
    wab = sb("wab", [32, 128], BF16)
    smallp = sb("smallp", [128, 64], F32)
    lbT = sb("lbT", [128, 2, 4], F32)
    omlbT = sb("omlbT", [128, 2, 4], F32)
    nwrep = sb("nwrep", [128, 128], F32)
    lqk = sb("lqk", [128, 4, 64], F32)
    ARENA_W = 10944
    arena = sb("arena", [128, ARENA_W], F32)

    pb = [st.enter_context(nc.psum_tensor(f"pb{i}", [128, 512], F32)) for i in range(8)]

    C_NBAF, C_NBAB, C_GLANW, C_HGNW, C_LAM, C_NLAM, C_T0, C_T1, C_T2, C_T3 = range(10)
    C_SS = 16
    C_RS = 32

    def col(c, n=1):
        return smallp[:, c:c + n]

    class Arena:
        def __init__(self):
            self.off = 0

        def take(self, shape, dt):
            n = 1
            for s in shape[1:]:
                n *= s
            words = (n * (2 if dt == BF16 else 4) + 3) // 4
            words = (words + 7) // 8 * 8
            o = self.off
            self.off += words
            assert self.off <= ARENA_W, (self.off, ARENA_W)
            ap = arena[:shape[0], o:o + words]
            if dt == BF16:
                ap = ap.bitcast(BF16)[:, 0:n]
            else:
                ap = ap[:, 0:n]
            if len(shape) == 3:
                ap = ap.rearrange("p (a b) -> p a b", b=shape[2])
            return ap

    def pbf(i):
        return pb[i][:, :].bitcast(BF16)

    dve = lambda fn, r=(), w=(): S.add("dve", fn, r, w)
    act = lambda fn, r=(), w=(): S.add("act", fn, r, w)
    pool = lambda fn, r=(), w=(): S.add("pool", fn, r, w)
    pe = lambda fn, r=(), w=(): S.add("pe", fn, r, w)

    A0 = Arena()
    c_iota = A0.take([128, 128], F32)
    c_z = A0.take([32, 128], F32)
    c_zb = A0.take([32, 128], BF16)
    c_t1 = A0.take([128, 128], F32)
    c_t2 = A0.take([128, 128], F32)
    c_ang = A0.take([128, L], F32)
    c_y = A0.take([128, L], F32)
    c_r = A0.take([128, L], F32)
    c_m = A0.take([128, L], F32)
    c_lg = A0.take([128, 2, 4], F32)
    c_lg2 = A0.take([128, 2, 4], F32)
    c_mx = A0.take([128, 2], F32)

    pool(lambda e: e.iota(c_iota, pattern=[[1, 128]], base=0, channel_multiplier=-1,
                          allow_small_or_imprecise_dtypes=True), w=["c_iota"])
    dve(lambda e: e.tensor_single_scalar(out=ident[:], in_=c_iota, scalar=0.0, op=ALU.is_equal), r=["c_iota"], w=["ident"])
    pool(lambda e: e.iota(c_z, pattern=[[1, 128]], base=0, channel_multiplier=-32,
                          allow_small_or_imprecise_dtypes=True), w=["c_z"])
    dve(lambda e: e.tensor_single_scalar(out=c_t1[0:32, :], in_=c_z, scalar=0.0, op=ALU.is_ge), r=["c_z"], w=["c_t1"])
    dve(lambda e: e.tensor_single_scalar(out=c_t2[0:32, :], in_=c_z, scalar=32.0, op=ALU.is_lt), r=["c_z"], w=["c_t2"])
    dve(lambda e: e.tensor_tensor(out=c_zb, in0=c_t1[0:32, :], in1=c_t2[0:32, :], op=ALU.mult), r=["c_t1", "c_t2"], w=["c_zb"])
    pe(lambda e: e.matmul(pb[0][:, 0:128], lhsT=c_zb, rhs=c_zb, start=True, stop=True), r=["c_zb"], w=["pb0"])
    dve(lambda e: e.tensor_single_scalar(out=c_t1, in_=c_iota, scalar=0.0, op=ALU.is_ge), r=["c_iota", "c_zb"], w=["c_t1"])
    dve(lambda e: e.tensor_single_scalar(out=c_t2, in_=c_iota, scalar=0.0, op=ALU.is_le), r=["c_iota", "c_zb"], w=["c_t2"])
    dve(lambda e: e.tensor_tensor(out=maskF[:], in0=pb[0][:, 0:128], in1=c_t1, op=ALU.mult), r=["pb0", "c_t1"], w=["maskF"])
    dve(lambda e: e.tensor_tensor(out=maskB[:], in0=pb[0][:, 0:128], in1=c_t2, op=ALU.mult), r=["pb0", "c_t2"], w=["maskB"])
    pool(lambda e: e.memset(bones[:], 0.0), w=["bones"])
    pool(lambda e: e.memset(bones[0:64, 0:64], 1.0 / 64), w=["bones"])
    pool(lambda e: e.memset(bones[64:128, 64:128], 1.0 / 64), w=["bones"])
    pool(lambda e: e.memset(rmask[:], 1.0), w=["rmask"])
    pool(lambda e: e.memset(rmask[:].rearrange("p (c k) -> p c k", k=32)[:, :, 0:1], 0.0), w=["rmask"])
    pool(lambda e: e.iota(col(C_T0), pattern=[[0, 1]], base=0, channel_multiplier=1,
                          allow_small_or_imprecise_dtypes=True), w=["cT0"])
    dve(lambda e: e.tensor_single_scalar(out=col(C_T1), in_=col(C_T0), scalar=32.0, op=ALU.is_ge), r=["cT0"], w=["cT1"])
    dve(lambda e: e.tensor_single_scalar(out=col(C_T2), in_=col(C_T0), scalar=64.0, op=ALU.is_ge), r=["cT0"], w=["cT2"])
    dve(lambda e: e.tensor_tensor(out=col(C_T1), in0=col(C_T1), in1=col(C_T2), op=ALU.add), r=["cT1", "cT2"], w=["cT1"])
    dve(lambda e: e.tensor_single_scalar(out=col(C_T2), in_=col(C_T0), scalar=96.0, op=ALU.is_ge), r=["cT0", "cT1"], w=["cT2"])
    dve(lambda e: e.tensor_tensor(out=col(C_T1), in0=col(C_T1), in1=col(C_T2), op=ALU.add), r=["cT1", "cT2"], w=["cT1"])
    dve(lambda e: e.scalar_tensor_tensor(out=col(C_T3), in0=col(C_T1), scalar=-32.0, in1=col(C_T0), op0=ALU.mult, op1=ALU.add),
        r=["cT0", "cT1"], w=["cT3"])
    act(lambda e: e.activation(out=col(C_T0), in_=col(C_T3), func=AF.Exp, scale=-math.log(10000.0) / 32.0), r=["cT3"], w=["cT0"])
    pool(lambda e: e.iota(c_ang, pattern=[[1, L]], base=0, channel_multiplier=0,
                          allow_small_or_imprecise_dtypes=True), w=["c_ang"])
    dve(lambda e: e.tensor_scalar(out=c_ang, in0=c_ang, scalar1=col(C_T0), scalar2=None, op0=ALU.mult), r=["c_ang", "cT0"], w=["c_ang"])
    MAGIC = 12582912.0
    for which, tab in ((0, sin_t), (1, cos_t)):
        dve(lambda e, which=which: e.tensor_scalar(out=c_y, in0=c_ang, scalar1=1.0 / (2 * math.pi), scalar2=0.25 * which,
                                                    op0=ALU.mult, op1=ALU.add), r=["c_ang", "c_y"], w=["c_y"])
        dve(lambda e: e.tensor_scalar(out=c_r, in0=c_y, scalar1=MAGIC, scalar2=None, op0=ALU.add), r=["c_y", "c_r"], w=["c_r"])
        dve(lambda e: e.tensor_scalar(out=c_r, in0=c_r, scalar1=MAGIC, scalar2=None, op0=ALU.subtract), r=["c_r"], w=["c_r"])
        dve(lambda e: e.tensor_tensor(out=c_y, in0=c_y, in1=c_r, op=ALU.subtract), r=["c_y", "c_r"], w=["c_y"])
        dve(lambda e: e.tensor_single_scalar(out=c_m, in_=c_y, scalar=0.5, op=ALU.is_gt), r=["c_y", "c_m"], w=["c_m"])
        dve(lambda e: e.tensor_tensor(out=c_y, in0=c_y, in1=c_m, op=ALU.subtract), r=["c_y", "c_m"], w=["c_y"])
        dve(lambda e: e.tensor_single_scalar(out=c_m, in_=c_y, scalar=-0.5, op=ALU.is_lt), r=["c_y", "c_m"], w=["c_m"])
        dve(lambda e: e.tensor_tensor(out=c_y, in0=c_y, in1=c_m, op=ALU.add), r=["c_y", "c_m"], w=["c_y"])
        act(lambda e, tab=tab: e.activation(out=tab[:], in_=c_y, func=AF.Sin, scale=6.28318), r=["c_y"], w=["tab%d" % which])
    for t_ in range(2):
        S.add("sp", lambda e, t_=t_: e.dma_start(out=c_lg[:, t_, :], in_=P["hgrn_lb_logits"][:, t_ * 128:(t_ + 1) * 128].rearrange("l p -> p l"),
                                                 allow_slow_non_contiguous=True), writes=["c_lg"], dma="prm")
    dve(lambda e: e.tensor_reduce(out=c_mx, in_=c_lg, axis=AX.X, op=ALU.max), r=["c_lg"], w=["c_mx"])
    dve(lambda e: e.tensor_tensor(out=c_lg2, in0=c_lg, in1=c_mx.unsqueeze(2).to_broadcast([128, 2, 4]), op=ALU.subtract),
        r=["c_lg", "c_mx"], w=["c_lg2"])
    act(lambda e: e.activation(out=c_lg2, in_=c_lg2, func=AF.Exp), r=["c_lg2"], w=["c_lg2"])
    dve(lambda e: e.tensor_reduce(out=c_mx, in_=c_lg2, axis=AX.X, op=ALU.add), r=["c_lg2", "c_mx"], w=["c_mx"])
    dve(lambda e: e.reciprocal(out=c_mx, in_=c_mx), r=["c_mx"], w=["c_mx"])
    dve(lambda e: e.tensor_tensor(out=c_lg2, in0=c_lg2, in1=c_mx.unsqueeze(2).to_broadcast([128, 2, 4]), op=ALU.mult),
        r=["c_lg2", "c_mx"], w=["c_lg2"])
    pool(lambda e: e.memset(lbT[:, :, 0:1], 0.0), w=["lbT"])
    for l in range(1, 4):
        dve(lambda e, l=l: e.tensor_tensor(out=lbT[:, :, l:l + 1], in0=lbT[:, :, l - 1:l], in1=c_lg2[:, :, l:l + 1], op=ALU.add),
            r=["c_lg2", "lbT"], w=["lbT"])
    dve(lambda e: e.tensor_scalar(out=omlbT[:], in0=lbT[:], scalar1=-1.0, scalar2=1.0, op0=ALU.mult, op1=ALU.add),
        r=["lbT"], w=["omlbT"])
    S.barrier()

    def load_w(slot, layer_, grp, ncols=512):
        for dc0 in (0, 4):
            S.add("pool", lambda e, dc0=dc0: e.dma_start(
                out=wbuf[slot][:, dc0:dc0 + 4, 0:ncols],
                in_=P["wpk"][layer_, grp].rearrange("p (dc f) -> p dc f", f=512)[:, dc0:dc0 + 4, 0:ncols]),
                writes=[("w", slot)], dma=f"w{slot}")

    def mm_fm(out_ap, slot, wc0, m, tok0, ntok, reads, writes, wsel=None):
        def fn(e):
            ins = None
            for dc in range(8):
                lhsT = wsel(dc) if wsel is not None else wbuf[slot][:, dc, wc0:wc0 + m]
                ins = e.matmul(out_ap, lhsT=lhsT, rhs=hT[:, dc, tok0:tok0 + ntok], start=(dc == 0), stop=(dc == 7))
            return ins
        blks = range(tok0 // 128, (tok0 + ntok) // 128)
        pe(fn, r=[("w", slot)] + [("hT", b) for b in blks] + list(reads), w=writes)

    def mm_tm(out_ap, slot, wc0, n, blk, writes):
        def fn(e):
            ins = None
            for dc in range(8):
                ins = e.matmul(out_ap, lhsT=hT[:, dc, blk * 128:(blk + 1) * 128], rhs=wbuf[slot][:, dc, wc0:wc0 + n],
                               start=(dc == 0), stop=(dc == 7))
            return ins
        pe(fn, r=[("w", slot), ("hT", blk)], w=writes)

    def rstd_cols(ss_ap, out_ap, inv_n, n, tag):
        act(lambda e: e.activation(out=out_ap, in_=ss_ap, func=AF.Ln, scale=inv_n, bias=epsc[:, 0:1]), r=[tag + "ss"], w=[tag + "rs"])
        act(lambda e: e.activation(out=out_ap, in_=out_ap, func=AF.Exp, scale=-0.5), r=[tag + "rs"], w=[tag + "rs"])

    epsc = sb("epsc", [128, 2], F32)
    pool(lambda e: e.memset(epsc[:, 0:1], EPS), w=["epsc"])
    pool(lambda e: e.memset(epsc[:, 1:2], 1.0), w=["epsc"])
    S.barrier()

    for seq in range(n_seq):
        for b in range(NB):
            S.add("sp", lambda e, b=b, seq=seq: e.dma_start(out=x_sb[:, b, :], in_=x_d[seq, b * 128:(b + 1) * 128, :]),
                  writes=[("x", b)], dma=f"xld{b}")
        for layer in range(n_layers):
            lam_init = 0.8 - 0.6 * math.exp(-0.3 * layer)
            prm = lambda fn, w: S.add("sp", fn, writes=w, dma="prm")
            for l_ in ([0, 1] if layer == 0 else [layer + 1]):
                if l_ < n_layers:
                    prm(lambda e, l_=l_: e.dma_start(out=gpreT[:, l_ % 2, :], in_=P["norm_pre"][l_].rearrange("(c p) -> p c", p=128),
                                                     allow_slow_non_contiguous=True), [("gpreT", l_ % 2)])
            prm(lambda e: e.dma_start(out=gpost[:], in_=P["norm_post"][layer].partition_broadcast(128)), ["gpost"])
            prm(lambda e: e.dma_start(out=nwrep[:], in_=P["diff_norm"][layer].partition_broadcast(128)), ["nwrep"])
            for i, nm in enumerate(("diff_lq1", "diff_lk1", "diff_lq2", "diff_lk2")):
                prm(lambda e, i=i, nm=nm: e.dma_start(out=lqk[:, i, :], in_=P[nm][layer].partition_broadcast(128)), ["lqk"])
            for c_, nm in ((C_NBAF, "gla_ba_fwd"), (C_NBAB, "gla_ba_bwd")):
                prm(lambda e, c_=c_, nm=nm: e.dma_start(out=col(c_), in_=P[nm][layer].rearrange("(p o) -> p o", o=1)), [("sp", c_)])
            for c_, nm in ((C_GLANW, "gla_norm"), (C_HGNW, "hgrn_norm")):
                for half in range(2):
                    prm(lambda e, c_=c_, nm=nm, half=half: e.dma_start(out=smallp[64 * half:64 * half + 64, c_:c_ + 1],
                                                                        in_=P[nm][layer].rearrange("(p o) -> p o", o=1)), [("sp", c_)])
            pool(lambda e: e.memset(waf[:], 0.0), w=["waf"])
            pool(lambda e: e.memset(wab[:], 0.0), w=["wab"])
            S.add("pool", lambda e: e.dma_start(out=waf[0:16, :], in_=P["gla_wa2_fwd"][layer]), reads=[], writes=["waf"], dma="prmc")
            S.add("pool", lambda e: e.dma_start(out=wab[16:32, :], in_=P["gla_wa2_bwd"][layer]), reads=[], writes=["wab"], dma="prmc")
            for c_ in (C_NBAF, C_NBAB):
                dve(lambda e, c_=c_: e.tensor_scalar(out=col(c_), in0=col(c_), scalar1=-1.0, scalar2=None, op0=ALU.mult),
                    r=[("sp", c_)], w=[("sp", c_)])
            dve(lambda e: e.tensor_scalar(out=nwrep[:], in0=nwrep[:], scalar1=1.0 - lam_init, scalar2=None, op0=ALU.mult),
                r=["nwrep"], w=["nwrep"])
            dve(lambda e: e.tensor_tensor(out=lqk[:, 0, :], in0=lqk[:, 0, :], in1=lqk[:, 1, :], op=ALU.mult), r=["lqk"], w=["lqk"])
            dve(lambda e: e.tensor_tensor(out=lqk[:, 2, :], in0=lqk[:, 2, :], in1=lqk[:, 3, :], op=ALU.mult), r=["lqk"], w=["lqk"])
            dve(lambda e: e.tensor_reduce(out=col(C_T0), in_=lqk[:, 0, :], axis=AX.X, op=ALU.add), r=["lqk"], w=["cT0"])
            dve(lambda e: e.tensor_reduce(out=col(C_T1), in_=lqk[:, 2, :], axis=AX.X, op=ALU.add), r=["lqk"], w=["cT1"])
            act(lambda e: e.activation(out=col(C_T0), in_=col(C_T0), func=AF.Exp), r=["cT0"], w=["cT0"])
            act(lambda e: e.activation(out=col(C_T1), in_=col(C_T1), func=AF.Exp), r=["cT1"], w=["cT1"])
            dve(lambda e: e.tensor_tensor(out=col(C_LAM), in0=col(C_T0), in1=col(C_T1), op=ALU.subtract), r=["cT0", "cT1"], w=["lam"])
            dve(lambda e: e.tensor_scalar(out=col(C_NLAM), in0=col(C_LAM), scalar1=-1.0, scalar2=-lam_init, op0=ALU.mult, op1=ALU.add),
                r=["lam"], w=["nlam"])
            S.barrier()

            C_PA = 48

            def phaseA_block(b, slot, hn, junk):
                act(lambda e: e.activation(out=junk, in_=x_sb[:, b, :], func=AF.Square, accum_out=col(C_PA + b)),
                    r=[("x", b), "junk"], w=["junk", ("rsA", b)])
                act(lambda e: e.activation(out=col(C_PA + b), in_=col(C_PA + b), func=AF.Ln, scale=1.0 / D, bias=epsc[:, 0:1]),
                    r=[("rsA", b)], w=[("rsA", b)])
                act(lambda e: e.activation(out=col(C_PA + b), in_=col(C_PA + b), func=AF.Exp, scale=-0.5),
                    r=[("rsA", b)], w=[("rsA", b)])
                dve(lambda e: e.tensor_scalar(out=hn[b % 2], in0=x_sb[:, b, :], scalar1=col(C_PA + b), scalar2=None, op0=ALU.mult),
                    r=[("x", b), ("rsA", b), ("hn", b % 2)], w=[("hn", b % 2)])
                bank = 6 + (b % 2)

                def tr(e):
                    ins = None
                    for dc in range(8):
                        ins = e.transpose(out=pbf(bank)[:, dc * 128:(dc + 1) * 128], in_=hn[b % 2][:, dc * 128:(dc + 1) * 128],
                                          identity=ident[:])
                    return ins
                pe(tr, r=[("hn", b % 2), "ident"], w=[("pb", bank)])
                dve(lambda e: e.tensor_tensor(
                    out=hT[:, :, b * 128:(b + 1) * 128], in0=pbf(bank).rearrange("p (c t) -> p c t", t=128),
                    in1=gpreT[:, slot, :].unsqueeze(2).to_broadcast([128, 8, 128]), op=ALU.mult),
                    r=[("pb", bank), ("gpreT", slot)], w=[("hT", b)])

            if layer == 0:
                A = Arena()
                hn = [A.take([128, D], BF16) for _ in range(2)]
                junk = A.take([128, D], BF16)
                for b in range(NB if stop >= 1 else 0):
                    phaseA_block(b, 0, hn, junk)
                S.barrier()

            for hp in range(2 if stop >= 2 else 0):
                A = Arena()
                kH = [A.take([128, L], BF16) for _ in range(2)]
                v1 = A.take([128, NB, 2 * 130], BF16)
                qH = [[A.take([128, 512], BF16) for _ in range(2)] for _ in range(2)]
                rtA = [A.take([128, 512], BF16) for _ in range(2)]
                rtB = [A.take([128, 512], BF16) for _ in range(2)]
                sg = [A.take([128, 4, 256], BF16) for _ in range(2)]
                T = [A.take([128, 512], F32) for _ in range(2)]
                On0 = A.take([128, 4, 132], F32)
                o_t = A.take([128, 4, 132], F32)
                PT = [A.take([128, 512], BF16) for _ in range(4)]
                ytok = [A.take([128, 128], BF16) for _ in range(4)]
                rcol = A.take([128, 16], F32)
                junk2 = A.take([128, 128], BF16)
                load_w(0, layer, 2 * hp)
                load_w(1, layer, 2 * hp + 1)
                pool(lambda e: e.memset(v1[:, :, :], 1.0), w=[("v1", b) for b in range(NB)])

                def wselAB(base, ab):
                    return lambda dc: wbuf[0][:, dc, base + ab * 128:base + ab * 128 + 128]

                rope_i = [0]

                def rope(psA, psB, tok0, dst, dcol0, key, wtag):
                    i = rope_i[0] % 2
                    rope_i[0] += 1
                    cs = cos_t[:, tok0:tok0 + 512]
                    sn = sin_t[:, tok0:tok0 + 512]
                    dve(lambda e: e.tensor_tensor(out=T[0], in0=pb[psA][:, :], in1=cs, op=ALU.mult), r=[("pb", psA), "T0"], w=["T0"])
                    dve(lambda e: e.tensor_tensor(out=T[1], in0=pb[psB][:, :], in1=sn, op=ALU.mult), r=[("pb", psB), "T1"], w=["T1"])
                    pool(lambda e: e.tensor_tensor(out=rtA[i], in0=T[0], in1=T[1], op=ALU.subtract), r=["T0", "T1", ("rtA", i)], w=[("rtA", i)])
                    dve(lambda e: e.tensor_tensor(out=T[0], in0=pb[psB][:, :], in1=cs, op=ALU.mult), r=[("pb", psB), "T0"], w=["T0"])
                    dve(lambda e: e.tensor_tensor(out=T[1], in0=pb[psA][:, :], in1=sn, op=ALU.mult), r=[("pb", psA), "T1"], w=["T1"])
                    pool(lambda e: e.tensor_tensor(out=rtB[i], in0=T[0], in1=T[1], op=ALU.add), r=["T0", "T1", ("rtB", i)], w=[("rtB", i)])
                    for hm in range(4):
                        hl_, c_ = hm // 2, hm % 2
                        for ab, rt, rtag in ((0, rtA, "rtA"), (1, rtB, "rtB")):
                            S.add("sp", lambda e, hm=hm, hl_=hl_, c_=c_, ab=ab, rt=rt: e.dma_start(
                                out=dst[hl_][64 * c_ + 32 * ab:64 * c_ + 32 * ab + 32, dcol0:dcol0 + 512],
                                in_=rt[i][32 * hm:32 * hm + 32, :]), reads=[(rtag, i)], writes=[wtag(hl_)], dma=key)

                for tg in range(NSEG):
                    mm_fm(pb[0][:, :], 0, 0, 128, tg * 512, 512, [], [("pb", 0)], wsel=wselAB(256, 0))
                    mm_fm(pb[1][:, :], 0, 0, 128, tg * 512, 512, [], [("pb", 1)], wsel=wselAB(256, 1))
                    rope(0, 1, tg * 512, kH, tg * 512, "kH", lambda hl_, tg=tg: ("kH", hl_, tg))
                for b in range(NB):
                    bank = 4 + (b % 2)
                    mm_tm(pb[bank][:, 0:256], 1, 0, 256, b, [("pb", bank)])
                    act(lambda e, b=b, bank=bank: e.copy(out=v1[:, b, :].rearrange("p (h c) -> p h c", c=130)[:, :, 0:128],
                                                          in_=pb[bank][:, 0:256].rearrange("p (h c) -> p h c", c=128)),
                        r=[("pb", bank)], w=[("v1", b)])
                def prep_q(qg):
                    par = qg % 2
                    mm_fm(pb[6][:, :], 0, 0, 128, qg * 512, 512, [], [("pb", 6)], wsel=wselAB(0, 0))
                    mm_fm(pb[7][:, :], 0, 0, 128, qg * 512, 512, [], [("pb", 7)], wsel=wselAB(0, 1))
                    rope(6, 7, qg * 512, qH[par], 0, "qH%d" % par, lambda hl_, par=par: ("qH", par, hl_))
                    for bl in range(4):
                        b = qg * 4 + bl
                        bank = 6 + bl % 2
                        mm_tm(pb[bank][:, 0:256], 1, 256, 256, b, [("pb", bank)])
                        act(lambda e, bl=bl, bank=bank, par=par: e.activation(out=sg[par][:, bl, :], in_=pb[bank][:, 0:256], func=AF.Silu),
                            r=[("pb", bank)], w=[("sg", par, bl)])
                        pool(lambda e, bl=bl, par=par: e.tensor_tensor(
                            out=sg[par][:, bl, :].rearrange("p (h v) -> p h v", v=128), in0=sg[par][:, bl, :].rearrange("p (h v) -> p h v", v=128),
                            in1=nwrep[:].unsqueeze(1).to_broadcast([128, 2, 128]), op=ALU.mult),
                            r=[("sg", par, bl), "nwrep"], w=[("sg", par, bl)])

                def emit_S(qg, hm, kc, sbank):
                    par = qg % 2

                    hl_, c_ = hm // 2, hm % 2

                    def smm(e):
                        return e.matmul(pb[sbank][:, :], lhsT=kH[hl_][64 * c_:64 * c_ + 64, kc * 128:(kc + 1) * 128],
                                        rhs=qH[par][hl_][64 * c_:64 * c_ + 64, :], start=True, stop=True, tile_position=(64 * c_, 0))
                    pe(smm, r=[("kH", hl_, kc // 4), ("qH", par, hl_)], w=[("pb", sbank)])

                def emit_PV(hl, kc, sbank, ptb):
                    act(lambda e: e.activation(out=PT[ptb], in_=pb[sbank][:, :], func=AF.Exp, scale=0.125),
                        r=[("pb", sbank)], w=[("PT", ptb)])

                    def pv(e):
                        ins = None
                        for qb in range(4):
                            ins = e.matmul(pb[2 + qb][:, 0:129], lhsT=PT[ptb][:, qb * 128:(qb + 1) * 128],
                                           rhs=v1[:, kc, hl * 130:hl * 130 + 129], start=(kc == 0), stop=(kc == NB - 1))
                        return ins
                    pe(pv, r=[("PT", ptb), ("v1", kc)], w=[("pb", 2 + qb) for qb in range(4)])

                def evacuate(qg, hl, c):
                    par = qg % 2
                    dst, dtag = (On0, "On0") if c == 0 else (o_t, "o_t")
                    for qb in range(4):
                        ob = 2 + qb
                        if qb % 2 == 0:
                            dve(lambda e, ob=ob, qb=qb: e.tensor_copy(out=dst[:, qb, 0:129], in_=pb[ob][:, 0:129]),
                                r=[("pb", ob)], w=[(dtag, qb)])
                        else:
                            act(lambda e, ob=ob, qb=qb: e.copy(out=dst[:, qb, 0:129], in_=pb[ob][:, 0:129]),
                                r=[("pb", ob)], w=[(dtag, qb)])
                    if c == 0:
                        return None
                    head = 2 * hp + hl
                    tb = 6 + hl

                    def st0():
                        for qb in range(4):
                            dve(lambda e, qb=qb: e.reciprocal(out=rcol[:, qb:qb + 1], in_=On0[:, qb, 128:129]), r=[("On0", qb)], w=[("rc", qb)])
                            dve(lambda e, qb=qb: e.reciprocal(out=rcol[:, 8 + qb:9 + qb], in_=o_t[:, qb, 128:129]), r=[("o_t", qb)], w=[("rc1", qb)])
                            dve(lambda e, qb=qb: e.tensor_tensor(out=rcol[:, 8 + qb:9 + qb], in0=rcol[:, 8 + qb:9 + qb], in1=col(C_NLAM),
                                                                 op=ALU.mult), r=[("rc1", qb), "nlam"], w=[("rc1", qb)])
                            dve(lambda e, qb=qb: e.tensor_scalar(out=On0[:, qb, 0:128], in0=On0[:, qb, 0:128], scalar1=rcol[:, qb:qb + 1],
                                                                 scalar2=None, op0=ALU.mult), r=[("On0", qb), ("rc", qb)], w=[("On0", qb)])
                            dve(lambda e, qb=qb: e.scalar_tensor_tensor(out=o_t[:, qb, 0:128], in0=o_t[:, qb, 0:128],
                                                                        scalar=rcol[:, 8 + qb:9 + qb], in1=On0[:, qb, 0:128],
                                                                        op0=ALU.mult, op1=ALU.add),
                                r=[("o_t", qb), ("rc1", qb), ("On0", qb)], w=[("o_t", qb)])
                            dve(lambda e, qb=qb: e.scalar_tensor_tensor(out=junk2, in0=o_t[:, qb, 0:128], scalar=1.0, in1=o_t[:, qb, 0:128],
                                                                        op0=ALU.mult, op1=ALU.mult, accum_out=rcol[:, 4 + qb:5 + qb]),
                                r=[("o_t", qb), "junk2"], w=["junk2", ("rss", qb)])

                    def st1():
                        act(lambda e: e.activation(out=rcol[:, 4:8], in_=rcol[:, 4:8], func=AF.Ln, scale=1.0 / 128, bias=epsc[:, 0:1]),
                            r=[("rss", q_) for q_ in range(4)], w=[("rss", q_) for q_ in range(4)])
                        act(lambda e: e.activation(out=rcol[:, 4:8], in_=rcol[:, 4:8], func=AF.Exp, scale=-0.5),
                            r=[("rss", q_) for q_ in range(4)], w=[("rss", q_) for q_ in range(4)])

                    def st2():
                        for qb in range(4):
                            dve(lambda e, qb=qb: e.scalar_tensor_tensor(out=ytok[qb], in0=o_t[:, qb, 0:128], scalar=rcol[:, 4 + qb:5 + qb],
                                                                        in1=sg[par][:, qb, hl * 128:(hl + 1) * 128],
                                                                        op0=ALU.mult, op1=ALU.mult),
                                r=[("o_t", qb), ("rss", qb), ("sg", par, qb), ("ytok", qb)], w=[("ytok", qb)])

                    def st3():
                        def tr4(e):
                            ins = None
                            for qb in range(4):
                                ins = e.transpose(out=pbf(tb)[:, qb * 128:(qb + 1) * 128], in_=ytok[qb], identity=ident[:])
                            return ins
                        pe(tr4, r=[("ytok", q_) for q_ in range(4)], w=[("pb", tb)])

                    def st4():
                        dve(lambda e: e.tensor_copy(out=yT[:, 2 + head, qg * 512:(qg + 1) * 512], in_=pbf(tb)[:, 0:512]),
                            r=[("pb", tb)], w=[("yT", 2 + head, qg * 4 + q_) for q_ in range(4)])
                    return [(2, st0), (5, st1), (8, st2), (11, st3), (14, st4)]

                its = [(qg, hl, c, kc) for qg in range(NSEG) for hl in range(2) for c in range(2) for kc in range(NB)]
                prep_q(0)
                deferred = []
                qg0, hl0, c0, kc0 = its[0]
                emit_S(qg0, 2 * hl0 + c0, kc0, 0)
                for i, (qg, hl, c, kc) in enumerate(its):
                    if i + 1 < len(its):
                        qg1, hl1, c1, kc1 = its[i + 1]
                        if qg1 != qg:
                            pass
                        emit_S(qg1, 2 * hl1 + c1, kc1, (i + 1) % 2)
                    emit_PV(hl, kc, i % 2, i % 4)
                    if kc == NB - 1:
                        t_ = evacuate(qg, hl, c)
                        if t_ is not None:
                            for dl, fn_ in t_:
                                deferred.append((i + dl, fn_))
                            deferred.sort(key=lambda z: z[0])
                    if (i % 64) == 24 and qg + 1 < NSEG:
                        prep_q(qg + 1)
                    while deferred and deferred[0][0] <= i:
                        deferred.pop(0)[1]()
                while deferred:
                    deferred.pop(0)[1]()
                S.barrier()

            for tile in range(DBG["tiles"] if stop >= 3 else 0):
                stg = DBG["stage"]
                is_gla = tile == 0
                NH = 4 if is_gla else 2
                dk = 32 if is_gla else 64
                npair = 2 if is_gla else 1
                sc = -1.0 / 16 if is_gla else 1.0
                qscale = 32 ** -0.5 if is_gla else 1.0
                nwc = C_GLANW if is_gla else C_HGNW
                A = Arena()
                qbT = A.take([128, L], BF16)
                kbT = A.take([128, L], BF16)
                Sb = A.take([128, 64, 64], BF16)
                vtok = A.take([128, NB, NH * 64], BF16)
                Sf = A.take([128, 17, 64], BF16)
                gcb = A.take([128, 64], F32)
                gcf = A.take([128, 64], F32)
                T = [A.take([128, 512], F32) for _ in range(4)]
                qfT = [A.take([128, 512], BF16) for _ in range(2)]
                kfT = [A.take([128, 512], BF16) for _ in range(2)]
                khT = A.take([128, 512], BF16)
                khtok = A.take([128, 4, 128], BF16)
                Am = [A.take([128, 128], BF16) for _ in range(4)]
                sq = A.take([128, 512], BF16)
                sgs = khT
                aT = sq[0:32, :]
                if is_gla:
                    load_w(0, layer, 4)
                    load_w(1, layer, 5, 288)
                    QC, KC, VC, GC, AC = 0, 128, 256, 0, 256
                else:
                    t_ = tile - 1
                    load_w(0, layer, 6 if t_ == 0 else 8)
                    if tile == 1:
                        load_w(1, layer, 7, 256)
                    QC, ZFC, ZBC, VC, GC = 0, 128, 256, 384, t_ * 128
                    lbc = lbT[:, t_, layer:layer + 1]
                    omlbc = omlbT[:, t_, layer:layer + 1]

                def chain(seg, bwd):
                    tok0 = seg * 512
                    ksrc = None
                    if is_gla:
                        mm_fm(pb[0][0:32, :], 1, AC, 32, tok0, 512, [], [("pb", 0)])
                        act(lambda e: e.copy(out=aT, in_=pb[0][0:32, :]), r=[("pb", 0), "sq"], w=["sq"])
                        wp = wab if bwd else waf
                        pe(lambda e: e.matmul(pb[1][:, :], lhsT=wp[:], rhs=aT, start=True, stop=True),
                           r=["sq", "waf", "wab"], w=[("pb", 1)])
                        nb_ = col(C_NBAB if bwd else C_NBAF)
                        act(lambda e: e.activation(out=T[1], in_=pb[1][:, :], func=AF.Exp, scale=-1.0, bias=nb_),
                            r=[("pb", 1), "T1", ("sp", C_NBAF), ("sp", C_NBAB)], w=["T1"])
                        act(lambda e: e.activation(out=T[0], in_=T[1], func=AF.Ln, bias=epsc[:, 1:2]), r=["T1", "T0"], w=["T0"])
                    else:
                        zc = ZBC if bwd else ZFC
                        mm_fm(pb[1][:, :], 0, zc, 128, tok0, 512, [], [("pb", 1)])
                        dve(lambda e: e.tensor_scalar(out=T[0], in0=pb[1][:, :], scalar1=-69.0, scalar2=None, op0=ALU.max),
                            r=[("pb", 1), "T0"], w=["T0"])
                        act(lambda e: e.activation(out=T[1], in_=T[0], func=AF.Sigmoid), r=["T0", "T1"], w=["T1"])
                        dve(lambda e: e.tensor_scalar(out=T[2], in0=T[1], scalar1=omlbc, scalar2=lbc, op0=ALU.mult, op1=ALU.add),
                            r=["T1", "T2", "lbT", "omlbT"], w=["T2"])
                        act(lambda e: e.activation(out=T[0], in_=T[2], func=AF.Ln), r=["T2", "T0"], w=["T0"])
                        pool(lambda e: e.tensor_scalar(out=T[3], in0=T[2], scalar1=-1.0, scalar2=1.0, op0=ALU.mult, op1=ALU.add),
                             r=["T2", "T3"], w=["T3"])
                        ksrc = T[3]
                    dve(lambda e: e.tensor_tensor_scan(out=T[1], data0=rmask[:], data1=T[0], initial=0.0, op0=ALU.mult, op1=ALU.add),
                        r=["T0", "T1", "rmask"], w=["T1"])
                    v3 = lambda t: t.rearrange("p (c k) -> p c k", k=32)
                    if bwd:
                        dve(lambda e: e.tensor_tensor(out=T[2], in0=T[0], in1=T[1], op=ALU.subtract), r=["T0", "T1", "T2"], w=["T2"])
                        dve(lambda e: e.tensor_tensor(out=v3(T[2]), in0=v3(T[2]), in1=v3(T[1])[:, :, 31:32].to_broadcast([128, 16, 32]),
                                                       op=ALU.add), r=["T1", "T2"], w=["T2"])
                        Bt, Btag, e_, ie_, etag, ietag = T[2], "T2", T[0], T[1], "T0", "T1"
                        gsel = v3(T[2])[:, :, 0]
                        gc = gcb
                    else:
                        Bt, Btag, e_, ie_, etag, ietag = T[1], "T1", T[0], T[2], "T0", "T2"
                        gsel = v3(T[1])[:, :, 31]
                        gc = gcf
                    act(lambda e: e.activation(out=gc[:, seg * 16:(seg + 1) * 16], in_=gsel, func=AF.Exp, scale=sc),
                        r=[Btag], w=[("gc", bwd, seg)])
                    act(lambda e: e.activation(out=e_, in_=Bt, func=AF.Exp, scale=sc), r=[Btag, etag], w=[etag])
                    act(lambda e: e.activation(out=ie_, in_=Bt, func=AF.Exp, scale=-sc), r=[Btag, ietag], w=[ietag])
                    return e_, etag, ie_, ietag, ksrc, gc

                def qk_products(seg, bwd, e_, etag, ie_, ietag, ksrc, q_out, k_out, qtag, ktag):
                    tok0 = seg * 512
                    mm_fm(pb[0][:, :], 0, QC, 128, tok0, 512, [], [("pb", 0)])
                    dve(lambda e: e.scalar_tensor_tensor(out=q_out, in0=pb[0][:, :], scalar=qscale, in1=e_, op0=ALU.mult, op1=ALU.mult),
                        r=[("pb", 0), etag] + qtag, w=qtag)
                    if is_gla:
                        mm_fm(pb[1][:, :], 0, KC, 128, tok0, 512, [], [("pb", 1)])
                        dve(lambda e: e.tensor_tensor(out=k_out, in0=pb[1][:, :], in1=ie_, op=ALU.mult),
                            r=[("pb", 1), ietag] + ktag, w=ktag)
                    else:
                        pool(lambda e: e.tensor_tensor(out=k_out, in0=ksrc, in1=ie_, op=ALU.mult), r=["T3", ietag] + ktag, w=ktag)

                UB = (2, 3, 7, 4)
                OB = (5, 6)

                def khat_and_U(seg, bwd, k_in, ktag, gc):
                    v3 = lambda t: t.rearrange("p (c k) -> p c k", k=32)
                    pool(lambda e: e.tensor_tensor(out=v3(khT), in0=v3(k_in),
                                                   in1=gc[:, seg * 16:(seg + 1) * 16].unsqueeze(2).to_broadcast([128, 16, 32]), op=ALU.mult),
                         r=ktag + [("gc", bwd, seg), "khT"], w=["khT"])

                    def tr(e):
                        ins = None
                        for bl in range(4):
                            ins = e.transpose(out=pbf(6)[:, bl * 128:(bl + 1) * 128], in_=khT[:, bl * 128:(bl + 1) * 128], identity=ident[:])
                        return ins
                    pe(tr, r=["khT"], w=[("pb", 6)])
                    act(lambda e: e.copy(out=khtok.rearrange("p a b -> p (a b)"), in_=pbf(6)[:, 0:512]), r=[("pb", 6), "khtok"], w=["khtok"])

                    def umm(e):
                        ins = None
                        for cl in range(16):
                            bl, j = cl // 4, cl % 4
                            b = seg * 4 + bl
                            for h in range(NH):
                                ins = e.matmul(pb[UB[j]][h * dk:(h + 1) * dk, bl * 64:bl * 64 + 64],
                                               lhsT=khtok[32 * j:32 * j + 32, bl, h * dk:(h + 1) * dk],
                                               rhs=vtok[32 * j:32 * j + 32, b, h * 64:(h + 1) * 64],
                                               start=True, stop=True, tile_position=(32 * j, h * dk))
                        return ins
                    pe(umm, r=["khtok"] + [("vtok", seg * 4 + bl) for bl in range(4)],
                       w=[("pb", u) for u in UB] + [("pbs", u, s_) for u in UB for s_ in range(2)])

                def stageA_b(seg_):
                    e_, etag, ie_, ietag, ksrc, gc = chain(seg_, True)
                    qk_products(seg_, True, e_, etag, ie_, ietag, ksrc, qbT[:, seg_ * 512:(seg_ + 1) * 512],
                                kbT[:, seg_ * 512:(seg_ + 1) * 512], [("qbT", seg_)], [("kbT", seg_)])

                def stageA_f(seg_):
                    par_ = seg_ % 2
                    e_, etag, ie_, ietag, ksrc, gc = chain(seg_, False)
                    qk_products(seg_, False, e_, etag, ie_, ietag, ksrc, qfT[par_], kfT[par_], [("qfT", par_)], [("kfT", par_)])

                pool(lambda e: e.memset(Sb[:, 63, :], 0.0), w=[("Sb", 63)])
                stageA_b(NSEG - 1)
                for seg in range(NSEG - 1, -1, -1):
                    if seg > 0:
                        stageA_b(seg - 1)
                    for bl in range(4):
                        b = seg * 4 + bl
                        bank = 5 + (bl % 2)
                        mm_tm(pb[bank][:, 0:NH * 64], 0, VC, NH * 64, b, [("pb", bank)])
                        act(lambda e, b=b, bank=bank: e.copy(out=vtok[:, b, :], in_=pb[bank][:, 0:NH * 64]), r=[("pb", bank)], w=[("vtok", b)])
                    khat_and_U(seg, True, kbT[:, seg * 512:(seg + 1) * 512], [("kbT", seg)], gcb)
                    for cl in range(15, -1, -1):
                        c = seg * 16 + cl
                        if c == 0:
                            continue
                        dve(lambda e, c=c, cl=cl: e.scalar_tensor_tensor(
                            out=Sb[:, c - 1, :], in0=Sb[:, c, :], scalar=gcb[:, c:c + 1],
                            in1=pb[UB[cl % 4]][:, (cl // 4) * 64:(cl // 4) * 64 + 64], op0=ALU.mult, op1=ALU.add),
                            r=[("Sb", c), ("gc", True, seg), ("pb", UB[cl % 4])], w=[("Sb", c - 1)])
                pool(lambda e: e.memset(Sf[:, 0, :], 0.0), w=[("Sf", 0)])
                stageA_f(0)
                for seg in range(NSEG if stg >= 5 else 0):
                    par = seg % 2
                    khat_and_U(seg, False, kfT[par], [("kfT", par)], gcf)
                    if seg > 0:
                        dve(lambda e: e.tensor_copy(out=Sf[:, 0, :], in_=Sf[:, 16, :]), r=[("Sf", 16)], w=[("Sf", 0)])
                    for cl in range(16):
                        c = seg * 16 + cl
                        dve(lambda e, c=c, cl=cl: e.scalar_tensor_tensor(
                            out=Sf[:, cl + 1, :], in0=Sf[:, cl, :], scalar=gcf[:, c:c + 1],
                            in1=pb[UB[cl % 4]][:, (cl // 4) * 64:(cl // 4) * 64 + 64], op0=ALU.mult, op1=ALU.add),
                            r=[("Sf", cl), ("gc", False, seg), ("pb", UB[cl % 4])], w=[("Sf", cl + 1)])
                    if seg + 1 < NSEG:
                        stageA_f(seg + 1)
                    def emit_AT(pr, bl, hl):
                        b = seg * 4 + bl
                        h = 2 * pr + hl
                        hb = h * dk
                        abank = UB[hb // 32]
                        ops_ = []
                        for d_ in range(2):
                            if d_ == 0:
                                kk, qq, ktg, qtg = kfT[par][hb:hb + dk, bl * 128:(bl + 1) * 128], \
                                    qfT[par][hb:hb + dk, bl * 128:(bl + 1) * 128], ("kfT", par), ("qfT", par)
                            else:
                                kk, qq, ktg, qtg = kbT[hb:hb + dk, b * 128:(b + 1) * 128], \
                                    qbT[hb:hb + dk, b * 128:(b + 1) * 128], ("kbT", seg), ("qbT", seg)
                            pe(lambda e, kk=kk, qq=qq, d_=d_: e.matmul(
                                pb[abank][:, d_ * 128:(d_ + 1) * 128], lhsT=kk, rhs=qq, start=True, stop=True,
                                tile_position=(hb, 0)), r=[ktg, qtg], w=[("pb", abank)])
                        for d_ in range(2):
                            slot = 2 * hl + d_
                            mk = maskF if d_ == 0 else maskB
                            dve(lambda e, slot=slot, mk=mk, d_=d_: e.tensor_tensor(
                                out=Am[slot], in0=pb[abank][:, d_ * 128:(d_ + 1) * 128], in1=mk[:], op=ALU.mult),
                                r=[("pb", abank), ("Am", slot)], w=[("Am", slot)])

                    def emit_omm(pr, bl, hl):
                        b = seg * 4 + bl
                        h = 2 * pr + hl
                        hb = h * dk
                        slots = (2 * hl, 2 * hl + 1)

                        def omm(e):
                            oc = pb[OB[hl]][64 * hl:64 * hl + 64, bl * 128:(bl + 1) * 128]
                            e.matmul(oc, lhsT=vtok[:, b, h * 64:(h + 1) * 64], rhs=Am[slots[0]], start=True, stop=False,
                                     tile_position=(0, 64 * hl))
                            e.matmul(oc, lhsT=vtok[:, b, h * 64:(h + 1) * 64], rhs=Am[slots[1]], start=False, stop=False,
                                     tile_position=(0, 64 * hl))
                            ins = None
                            for j in range(4):
                                cl = bl * 4 + j
                                e.matmul(pb[OB[hl]][64 * hl:64 * hl + 64, bl * 128 + 32 * j:bl * 128 + 32 * j + 32],
                                         lhsT=Sf[hb:hb + dk, cl, :], rhs=qfT[par][hb:hb + dk, bl * 128 + 32 * j:bl * 128 + 32 * j + 32],
                                         start=False, stop=False, tile_position=(hb, 64 * hl))
                            for j in range(4):
                                c = b * 4 + j
                                ins = e.matmul(pb[OB[hl]][64 * hl:64 * hl + 64, bl * 128 + 32 * j:bl * 128 + 32 * j + 32],
                                               lhsT=Sb[hb:hb + dk, c, :], rhs=qbT[hb:hb + dk, b * 128 + 32 * j:b * 128 + 32 * j + 32],
                                               start=False, stop=(j == 3), tile_position=(hb, 64 * hl))
                            return ins
                        pe(omm, r=[("vtok", b), ("Am", slots[0]), ("Am", slots[1]), ("qfT", par), ("qbT", seg)]
                           + [("Sf", bl * 4 + j) for j in range(4)] + [("Sb", b * 4 + j) for j in range(4)], w=[("pb", OB[hl])])

                    units = [(pr, bl, hl) for pr in range(npair if stg >= 6 else 0) for bl in range(4) for hl in range(2)]
                    if units:
                        emit_AT(*units[0])
                    for ui, (pr, bl, hl) in enumerate(units):
                        if ui + 1 < len(units):
                            emit_AT(*units[ui + 1])
                        emit_omm(pr, bl, hl)
                        if not (bl == 3 and hl == 1):
                            continue
                        ym = (0 if is_gla else 5 + tile) + pr
                        if stg < 8:
                            continue
                        for hl in range(2):
                            act(lambda e, hl=hl: e.activation(out=sq[64 * hl:64 * hl + 64, :], in_=pb[OB[hl]][64 * hl:64 * hl + 64, :],
                                                              func=AF.Square), r=[("pb", OB[hl]), "sq"], w=["sq"])
                        pe(lambda e: e.matmul(pb[0][:, :], lhsT=bones[:], rhs=sq, start=True, stop=True), r=["sq"], w=[("pb", 0)])
                        act(lambda e: e.activation(out=T[3], in_=pb[0][:, :], func=AF.Ln, bias=epsc[:, 0:1]), r=[("pb", 0), "T3"], w=["T3"])
                        act(lambda e: e.activation(out=T[3], in_=T[3], func=AF.Exp, scale=-0.5), r=["T3"], w=["T3"])
                        for hl in range(2):
                            dve(lambda e, hl=hl: e.scalar_tensor_tensor(
                                out=T[0][64 * hl:64 * hl + 64, :], in0=pb[OB[hl]][64 * hl:64 * hl + 64, :],
                                scalar=smallp[64 * hl:64 * hl + 64, nwc:nwc + 1], in1=T[3][64 * hl:64 * hl + 64, :],
                                op0=ALU.mult, op1=ALU.mult), r=[("pb", OB[hl]), "T3", "T0", ("sp", nwc)], w=["T0"])
                        mm_fm(pb[1][:, :], 1, GC + pr * 128, 128, seg * 512, 512, [], [("pb", 1)])
                        act(lambda e: e.activation(out=sgs, in_=pb[1][:, :], func=AF.Silu), r=[("pb", 1), "khT"], w=["khT"])
                        pool(lambda e, ym=ym, seg=seg: e.tensor_tensor(out=yT[:, ym, seg * 512:(seg + 1) * 512], in0=T[0], in1=sgs, op=ALU.mult),
                             r=["T0", "khT"], w=[("yT", ym, seg * 4 + i) for i in range(4)])
                S.barrier()

            A = Arena()
            T = [A.take([128, 512], F32) for _ in range(2)]
            junk3 = A.take([128, 512], BF16)
            fuseA = layer + 1 < n_layers
            if fuseA:
                hn = [A.take([128, D], BF16) for _ in range(2)]
                junk = A.take([128, D], BF16)
            load_w(0, layer, 9)
            load_w(1, layer, 10)
            for b in range(NB if stop >= 4 else 0):
                for half in range(2):
                    bank = 2 * (b % 2) + half

                    def omm2(e, b=b, half=half, bank=bank):
                        ins = None
                        for mc in range(8):
                            ins = e.matmul(pb[bank][:, :], lhsT=yT[:, mc, b * 128:(b + 1) * 128], rhs=wbuf[half][:, mc, :],
                                           start=(mc == 0), stop=(mc == 7))
                        return ins
                    pe(omm2, r=[("w", half)] + [("yT", m, b) for m in range(8)], w=[("pb", bank)])
                    act(lambda e, b=b, half=half, bank=bank: e.activation(out=junk3, in_=pb[bank][:, :], func=AF.Square,
                                                                          accum_out=col(C_SS + 2 * (b % 2) + half)),
                        r=[("pb", bank), "junk3"], w=["junk3", ("ss2", b % 2, half)])
                pc = C_SS + 2 * (b % 2)
                rc_ = C_RS + (b % 2)
                dve(lambda e, pc=pc, rc_=rc_: e.tensor_tensor(out=col(rc_), in0=col(pc), in1=col(pc + 1), op=ALU.add),
                    r=[("ss2", b % 2, 0), ("ss2", b % 2, 1)], w=[("rs2", b % 2)])
                act(lambda e, rc_=rc_: e.activation(out=col(rc_), in_=col(rc_), func=AF.Ln, scale=1.0 / D, bias=epsc[:, 0:1]),
                    r=[("rs2", b % 2)], w=[("rs2", b % 2)])
                act(lambda e, rc_=rc_: e.activation(out=col(rc_), in_=col(rc_), func=AF.Exp, scale=-0.5), r=[("rs2", b % 2)], w=[("rs2", b % 2)])
                for half in range(2):
                    bank = 2 * (b % 2) + half
                    dve(lambda e, half=half, bank=bank, rc_=rc_: e.scalar_tensor_tensor(
                        out=T[half], in0=pb[bank][:, :], scalar=col(rc_), in1=gpost[:, half * 512:(half + 1) * 512],
                        op0=ALU.mult, op1=ALU.mult), r=[("pb", bank), ("rs2", b % 2), "gpost", ("Tn", half)], w=[("Tn", half)])
                    pool(lambda e, b=b, half=half: e.tensor_tensor(out=x_sb[:, b, half * 512:(half + 1) * 512],
                                                                   in0=x_sb[:, b, half * 512:(half + 1) * 512], in1=T[half], op=ALU.add),
                         r=[("Tn", half), ("x", b)], w=[("x", b)])
                if fuseA and b >= 3:
                    phaseA_block(b - 3, (layer + 1) % 2, hn, junk)
            if fuseA:
                for b_ in range(NB - 3, NB):
                    phaseA_block(b_, (layer + 1) % 2, hn, junk)
            if dbg and seq == 0 and layer == 0:
                S.add("sp", lambda e: e.dma_start(out=dbg_y, in_=yT[:]), reads=[("yT", m, b) for m in range(8) for b in range(NB)], dma="dbg")
                S.add("sp", lambda e: e.dma_start(out=dbg_h, in_=hT[:]), reads=[("hT", b) for b in range(NB)], dma="dbg")
            S.barrier()
        for b in range(NB):
            S.add("sp", lambda e, b=b, seq=seq: e.dma_start(out=out_d[seq, b * 128:(b + 1) * 128, :], in_=x_sb[:, b, :]),
                  reads=[("x", b)], dma=f"xst{b}")
    S.emit(st)
    st.close()
    return nc


def _pack_weights(w_in, w_out):
    def cols_qk(base):
        idx = []
        for ab in range(2):
            for hm in range(4):
                s = base + hm * 64 + ab * 32
                idx.extend(range(s, s + 32))
        return idx
    groups = []
    for hp in range(2):
        groups.append(cols_qk(800 + hp * 256) + cols_qk(1312 + hp * 256))
        groups.append(list(range(1824 + hp * 256, 2080 + hp * 256)) + list(range(2336 + hp * 256, 2592 + hp * 256)))
    groups.append(list(range(0, 512)))
    groups.append(list(range(512, 800)))
    for t_ in range(2):
        g = []
        for b0 in (2848, 3104, 3360, 3616):
            g.extend(range(b0 + t_ * 128, b0 + t_ * 128 + 128))
        groups.append(g)
        if t_ == 0:
            groups.append(list(range(3872, 4128)))
    out = np.zeros((4, 11, 128, 8, 512), np.float32)
    for l in range(4):
        wl = w_in[l].reshape(8, 128, IN_W)
        for gi, g in enumerate(groups):
            out[l, gi, :, :, :len(g)] = wl[:, :, g].transpose(1, 0, 2)
        wo = w_out[l].reshape(8, 128, 1024)
        out[l, 9] = wo[:, :, 0:512].transpose(1, 0, 2)
        out[l, 10] = wo[:, :, 512:1024].transpose(1, 0, 2)
    return out.reshape(4, 11, 128, 4096)


_NC_CACHE = {}


def kernel(**inputs):
    x = np.ascontiguousarray(np.asarray(inputs["x"], dtype=np.float32))
    B = x.shape[0]
    per = B // N_CORES
    if "nc" not in _NC_CACHE:
        _NC_CACHE["nc"] = build(4, per)
    nc = _NC_CACHE["nc"]
    params = {name: np.ascontiguousarray(np.asarray(inputs[name], dtype=np.float32)) for name, _ in PARAM_SPECS if name != "wpk"}
    params["wpk"] = _pack_weights(np.asarray(inputs["w_in"], dtype=np.float32), np.asarray(inputs["w_out"], dtype=np.float32))
    in_maps = []
    for c in range(N_CORES):
        m = {"x": x[c * per:(c + 1) * per]}
        m.update(params)
        in_maps.append(m)
    res = run_bass_kernel_spmd(nc, in_maps, core_ids=list(range(N_CORES)))
    out = np.concatenate([np.asarray(r["out"]) for r in res.results], axis=0)
    return out.astype(np.float32)
```

```python
import math
import bisect
import types
from contextlib import ExitStack

import numpy as np
import concourse.bass as bass
import concourse.mybir as mybir
from concourse.bass_utils import run_bass_kernel_spmd

F32 = mybir.dt.float32
BF16 = mybir.dt.bfloat16
AF = mybir.ActivationFunctionType
ALU = mybir.AluOpType
AX = mybir.AxisListType

L = 2048
D = 1024
NB = 16
NSEG = 4
IN_W = 4128
EPS = 1e-6
N_CORES = 8

ENGS = ("pe", "act", "dve", "pool", "sp")
SEM_LIMIT = 20000


class Op:
    __slots__ = ("eng", "fn", "reads", "writes", "gi", "dma", "deps", "bdeps", "sig", "sigidx", "waits", "dmaval")

    def __init__(self, eng, fn, reads, writes, dma):
        self.eng = eng
        self.fn = fn
        self.reads = reads
        self.writes = writes
        self.dma = dma
        self.deps = set()
        self.sig = False
        self.sigidx = None
        self.waits = []
        self.dmaval = None


def _freeze(fn):
    if fn.__closure__ is None:
        return fn
    cells = []
    for c in fn.__closure__:
        try:
            cells.append(types.CellType(c.cell_contents))
        except ValueError:
            cells.append(c)
    return types.FunctionType(fn.__code__, fn.__globals__, fn.__name__, fn.__defaults__, tuple(cells))


class Sched:
    def __init__(self, nc):
        self.nc = nc
        self.ops = []
        self.last_writer = {}
        self.readers = {}
        self.barrier_ops = []

    def add(self, eng, fn, reads=(), writes=(), dma=None):
        op = Op(eng, _freeze(fn), tuple(reads), tuple(writes), dma)
        op.gi = len(self.ops)
        deps = op.deps
        for r in op.reads:
            w = self.last_writer.get(r)
            if w is not None:
                deps.add(w)
        for r in op.writes:
            w = self.last_writer.get(r)
            if w is not None and not (dma is not None and w.dma == dma):
                deps.add(w)
            for rd in self.readers.get(r, ()):
                deps.add(rd)
        op.bdeps = self.barrier_ops
        deps.discard(op)
        for r in op.writes:
            self.last_writer[r] = op
            self.readers[r] = []
        for r in op.reads:
            self.readers.setdefault(r, []).append(op)
        self.ops.append(op)
        return op

    def barrier(self):
        last = {}
        for op in self.ops:
            key = ("dma", op.dma) if op.dma is not None else ("eng", op.eng)
            last[key] = op
        self.barrier_ops = list(last.values())
        self.last_writer = {}
        self.readers = {}

    def emit(self, stack):
        nc = self.nc
        ops = self.ops

        def unsynced(d, op):
            return d.dma is None and op.dma is None and d.eng == "pe" and op.eng == "pe"

        for op in ops:
            for d in op.deps:
                if not unsynced(d, op):
                    d.sig = True
            for d in op.bdeps:
                if d is not op and d.dma is None:
                    d.sig = True
        sigcount = {e: 0 for e in ENGS}
        dmacount = {}
        dma_hist = {}
        for op in ops:
            if op.dma is not None:
                dmacount[op.dma] = dmacount.get(op.dma, 0) + 16
                op.dmaval = dmacount[op.dma]
                dma_hist.setdefault(op.dma, []).append((op.gi, op.dmaval))
            elif op.sig:
                sigcount[op.eng] += 1
                op.sigidx = sigcount[op.eng]
        sems = {e: [stack.enter_context(nc.semaphore(f"s_{e}_{i}"))
                    for i in range(sigcount[e] // SEM_LIMIT + 1)] for e in ENGS}
        dsems = {}
        for k, v in dmacount.items():
            assert v < 30000, (k, v)
            dsems[k] = stack.enter_context(nc.semaphore(f"d_{k}"))

        def semval(eng, idx):
            return sems[eng][(idx - 1) // SEM_LIMIT], (idx - 1) % SEM_LIMIT + 1

        waited = {e: {s: 0 for s in ENGS} for e in ENGS}
        dwaited = {e: {} for e in ENGS}
        for op in ops:
            need = {}
            dneed = {}
            for d in op.deps:
                if d.dma is not None:
                    hist = dma_hist[d.dma]
                    pos = bisect.bisect_left(hist, (op.gi, 0)) - 1
                    val = hist[pos][1]
                    assert val >= d.dmaval
                    dneed[d.dma] = max(dneed.get(d.dma, 0), val)
                elif not unsynced(d, op):
                    need[d.eng] = max(need.get(d.eng, 0), d.sigidx)
            for d in op.bdeps:
                if d is op:
                    continue
                if d.dma is not None:
                    dneed[d.dma] = max(dneed.get(d.dma, 0), d.dmaval)
                else:
                    need[d.eng] = max(need.get(d.eng, 0), d.sigidx)
            w = []
            for s, idx in need.items():
                if idx > waited[op.eng][s]:
                    waited[op.eng][s] = idx
                    w.append(semval(s, idx))
            for k, val in dneed.items():
                if val > dwaited[op.eng].get(k, 0):
                    dwaited[op.eng][k] = val
                    w.append((dsems[k], val))
            op.waits = w

        block = stack.enter_context(nc.Block())
        per_eng = {e: [op for op in ops if op.eng == e] for e in ENGS}

        def run(engname, engine):
            lst = per_eng[engname]
            for op in lst:
                for (sem, val) in op.waits:
                    engine.wait_ge(sem, val)
                ins = op.fn(engine)
                if op.dma is not None:
                    ins.then_inc(dsems[op.dma], 16)
                elif op.sig:
                    ins.then_inc(semval(engname, op.sigidx)[0], 1)
            done = {}
            for op in lst:
                if op.dma is not None:
                    done[op.dma] = dmacount[op.dma]
            for k, val in done.items():
                engine.wait_ge(dsems[k], val)

        @block.tensor
        def _(e):
            run("pe", e)

        @block.scalar
        def _(e):
            run("act", e)

        @block.vector
        def _(e):
            run("dve", e)

        @block.gpsimd
        def _(e):
            run("pool", e)

        @block.sync
        def _(e):
            run("sp", e)


PARAM_SPECS = [
    ("norm_pre", [4, 1024]), ("norm_post", [4, 1024]), ("wpk", [4, 11, 128, 4096]),
    ("gla_wa2_fwd", [4, 16, 128]), ("gla_ba_fwd", [4, 128]), ("gla_wa2_bwd", [4, 16, 128]), ("gla_ba_bwd", [4, 128]),
    ("gla_norm", [4, 64]), ("diff_lq1", [4, 64]), ("diff_lk1", [4, 64]), ("diff_lq2", [4, 64]), ("diff_lk2", [4, 64]),
    ("diff_norm", [4, 128]), ("hgrn_lb_logits", [4, 256]), ("hgrn_norm", [4, 64]),
]


DBG = {"tiles": 3, "stage": 99}


def build(n_layers=4, n_seq=2, dbg=False, stop=9):
    nc = bass.Bass("TRN2", target_bir_lowering=False)
    x_d = nc.dram_tensor("x", [n_seq, L, D], F32, kind="ExternalInput").ap()
    out_d = nc.dram_tensor("out", [n_seq, L, D], F32, kind="ExternalOutput").ap()
    P = {}
    for name, shp in PARAM_SPECS:
        P[name] = nc.dram_tensor(name, shp, F32, kind="ExternalInput").ap()
    if dbg:
        dbg_y = nc.dram_tensor("dbg_y", [128, 8, L], BF16, kind="ExternalOutput").ap()
        dbg_h = nc.dram_tensor("dbg_h", [128, 8, L], BF16, kind="ExternalOutput").ap()

    st = ExitStack()
    S = Sched(nc)

    def sb(name, shape, dt):
        return st.enter_context(nc.sbuf_tensor(name, shape, dt))

    x_sb = sb("x_sb", [128, NB, D], F32)
    hT = sb("hT", [128, 8, L], BF16)
    yT = sb("yT", [128, 8, L], BF16)
    wbuf = [sb(f"wbuf{i}", [128, 8, 512], BF16) for i in range(2)]
    gpost = sb("gpost", [128, D], F32)
    cos_t = sb("cos_t", [128, L], BF16)
    sin_t = sb("sin_t", [128, L], BF16)
    ident = sb("ident", [128, 128], BF16)
    maskF = sb("maskF", [128, 128], BF16)
    maskB = sb("maskB", [128, 128], BF16)
    bones = sb("bones", [128, 128], BF16)
    rmask = sb("rmask", [128, 512], BF16)
    gpreT = sb("gpreT", [128, 8], F32)
    waf = sb("waf", [32, 128], BF16)
    wab = sb("wab", [32, 128], BF16)
    smallp = sb("smallp", [128, 64], F32)
    lbT = sb("lbT", [128, 2, 4], F32)
    omlbT = sb("omlbT", [128, 2, 4], F32)
    nwrep = sb("nwrep", [128, 128], F32)
    lqk = sb("lqk", [128, 4, 64], F32)
    ARENA_W = 10944
    arena = sb("arena", [128, ARENA_W], F32)

    pb = [st.enter_context(nc.psum_tensor(f"pb{i}", [128, 512], F32)) for i in range(8)]

    C_NBAF, C_NBAB, C_GLANW, C_HGNW, C_LAM, C_NLAM, C_T0, C_T1, C_T2, C_T3 = range(10)
    C_SS = 16
    C_RS = 32

    def col(c, n=1):
        return smallp[:, c:c + n]

    class Arena:
        def __init__(self):
            self.off = 0

        def take(self, shape, dt):
            n = 1
            for s in shape[1:]:
                n *= s
            words = (n * (2 if dt == BF16 else 4) + 3) // 4
            words = (words + 7) // 8 * 8
            o = self.off
            self.off += words
            assert self.off <= ARENA_W, (self.off, ARENA_W)
            ap = arena[:shape[0], o:o + words]
            if dt == BF16:
                ap = ap.bitcast(BF16)[:, 0:n]
            else:
                ap = ap[:, 0:n]
            if len(shape) == 3:
                ap = ap.rearrange("p (a b) -> p a b", b=shape[2])
            return ap

    def pbf(i):
        return pb[i][:, :].bitcast(BF16)

    dve = lambda fn, r=(), w=(): S.add("dve", fn, r, w)
    act = lambda fn, r=(), w=(): S.add("act", fn, r, w)
    pool = lambda fn, r=(), w=(): S.add("pool", fn, r, w)
    pe = lambda fn, r=(), w=(): S.add("pe", fn, r, w)

    A0 = Arena()
    c_iota = A0.take([128, 128], F32)
    c_z = A0.take([32, 128], F32)
    c_zb = A0.take([32, 128], BF16)
    c_t1 = A0.take([128, 128], F32)
    c_t2 = A0.take([128, 128], F32)
    c_ang = A0.take([128, L], F32)
    c_y = A0.take([128, L], F32)
    c_r = A0.take([128, L], F32)
    c_m = A0.take([128, L], F32)
    c_lg = A0.take([128, 2, 4], F32)
    c_lg2 = A0.take([128, 2, 4], F32)
    c_mx = A0.take([128, 2], F32)

    pool(lambda e: e.iota(c_iota, pattern=[[1, 128]], base=0, channel_multiplier=-1,
                          allow_small_or_imprecise_dtypes=True), w=["c_iota"])
    dve(lambda e: e.tensor_single_scalar(out=ident[:], in_=c_iota, scalar=0.0, op=ALU.is_equal), r=["c_iota"], w=["ident"])
    pool(lambda e: e.iota(c_z, pattern=[[1, 128]], base=0, channel_multiplier=-32,
                          allow_small_or_imprecise_dtypes=True), w=["c_z"])
    dve(lambda e: e.tensor_single_scalar(out=c_t1[0:32, :], in_=c_z, scalar=0.0, op=ALU.is_ge), r=["c_z"], w=["c_t1"])
    dve(lambda e: e.tensor_single_scalar(out=c_t2[0:32, :], in_=c_z, scalar=32.0, op=ALU.is_lt), r=["c_z"], w=["c_t2"])
    dve(lambda e: e.tensor_tensor(out=c_zb, in0=c_t1[0:32, :], in1=c_t2[0:32, :], op=ALU.mult), r=["c_t1", "c_t2"], w=["c_zb"])
    pe(lambda e: e.matmul(pb[0][:, 0:128], lhsT=c_zb, rhs=c_zb, start=True, stop=True), r=["c_zb"], w=["pb0"])
    dve(lambda e: e.tensor_single_scalar(out=c_t1, in_=c_iota, scalar=0.0, op=ALU.is_ge), r=["c_iota", "c_zb"], w=["c_t1"])
    dve(lambda e: e.tensor_single_scalar(out=c_t2, in_=c_iota, scalar=0.0, op=ALU.is_le), r=["c_iota", "c_zb"], w=["c_t2"])
    dve(lambda e: e.tensor_tensor(out=maskF[:], in0=pb[0][:, 0:128], in1=c_t1, op=ALU.mult), r=["pb0", "c_t1"], w=["maskF"])
    dve(lambda e: e.tensor_tensor(out=maskB[:], in0=pb[0][:, 0:128], in1=c_t2, op=ALU.mult), r=["pb0", "c_t2"], w=["maskB"])
    pool(lambda e: e.memset(bones[:], 0.0), w=["bones"])
    pool(lambda e: e.memset(bones[0:64, 0:64], 1.0 / 64), w=["bones"])
    pool(lambda e: e.memset(bones[64:128, 64:128], 1.0 / 64), w=["bones"])
    pool(lambda e: e.memset(rmask[:], 1.0), w=["rmask"])
    pool(lambda e: e.memset(rmask[:].rearrange("p (c k) -> p c k", k=32)[:, :, 0:1], 0.0), w=["rmask"])
    pool(lambda e: e.iota(col(C_T0), pattern=[[0, 1]], base=0, channel_multiplier=1,
                          allow_small_or_imprecise_dtypes=True), w=["cT0"])
    dve(lambda e: e.tensor_single_scalar(out=col(C_T1), in_=col(C_T0), scalar=32.0, op=ALU.is_ge), r=["cT0"], w=["cT1"])
    dve(lambda e: e.tensor_single_scalar(out=col(C_T2), in_=col(C_T0), scalar=64.0, op=ALU.is_ge), r=["cT0"], w=["cT2"])
    dve(lambda e: e.tensor_tensor(out=col(C_T1), in0=col(C_T1), in1=col(C_T2), op=ALU.add), r=["cT1", "cT2"], w=["cT1"])
    dve(lambda e: e.tensor_single_scalar(out=col(C_T2), in_=col(C_T0), scalar=96.0, op=ALU.is_ge), r=["cT0", "cT1"], w=["cT2"])
    dve(lambda e: e.tensor_tensor(out=col(C_T1), in0=col(C_T1), in1=col(C_T2), op=ALU.add), r=["cT1", "cT2"], w=["cT1"])
    dve(lambda e: e.scalar_tensor_tensor(out=col(C_T3), in0=col(C_T1), scalar=-32.0, in1=col(C_T0), op0=ALU.mult, op1=ALU.add),
        r=["cT0", "cT1"], w=["cT3"])
    act(lambda e: e.activation(out=col(C_T0), in_=col(C_T3), func=AF.Exp, scale=-math.log(10000.0) / 32.0), r=["cT3"], w=["cT0"])
    pool(lambda e: e.iota(c_ang, pattern=[[1, L]], base=0, channel_multiplier=0,
                          allow_small_or_imprecise_dtypes=True), w=["c_ang"])
    dve(lambda e: e.tensor_scalar(out=c_ang, in0=c_ang, scalar1=col(C_T0), scalar2=None, op0=ALU.mult), r=["c_ang", "cT0"], w=["c_ang"])
    MAGIC = 12582912.0
    for which, tab in ((0, sin_t), (1, cos_t)):
        dve(lambda e, which=which: e.tensor_scalar(out=c_y, in0=c_ang, scalar1=1.0 / (2 * math.pi), scalar2=0.25 * which,
                                                    op0=ALU.mult, op1=ALU.add), r=["c_ang", "c_y"], w=["c_y"])
        dve(lambda e: e.tensor_scalar(out=c_r, in0=c_y, scalar1=MAGIC, scalar2=None, op0=ALU.add), r=["c_y", "c_r"], w=["c_r"])
        dve(lambda e: e.tensor_scalar(out=c_r, in0=c_r, scalar1=MAGIC, scalar2=None, op0=ALU.subtract), r=["c_r"], w=["c_r"])
        dve(lambda e: e.tensor_tensor(out=c_y, in0=c_y, in1=c_r, op=ALU.subtract), r=["c_y", "c_r"], w=["c_y"])
        dve(lambda e: e.tensor_single_scalar(out=c_m, in_=c_y, scalar=0.5, op=ALU.is_gt), r=["c_y", "c_m"], w=["c_m"])
        dve(lambda e: e.tensor_tensor(out=c_y, in0=c_y, in1=c_m, op=ALU.subtract), r=["c_y", "c_m"], w=["c_y"])
        dve(lambda e: e.tensor_single_scalar(out=c_m, in_=c_y, scalar=-0.5, op=ALU.is_lt), r=["c_y", "c_m"], w=["c_m"])
        dve(lambda e: e.tensor_tensor(out=c_y, in0=c_y, in1=c_m, op=ALU.add), r=["c_y", "c_m"], w=["c_y"])
        act(lambda e, tab=tab: e.activation(out=tab[:], in_=c_y, func=AF.Sin, scale=6.28318), r=["c_y"], w=["tab%d" % which])
    for t_ in range(2):
        S.add("sp", lambda e, t_=t_: e.dma_start(out=c_lg[:, t_, :], in_=P["hgrn_lb_logits"][:, t_ * 128:(t_ + 1) * 128].rearrange("l p -> p l"),
                                                 allow_slow_non_contiguous=True), writes=["c_lg"], dma="prm")
    dve(lambda e: e.tensor_reduce(out=c_mx, in_=c_lg, axis=AX.X, op=ALU.max), r=["c_lg"], w=["c_mx"])
    dve(lambda e: e.tensor_tensor(out=c_lg2, in0=c_lg, in1=c_mx.unsqueeze(2).to_broadcast([128, 2, 4]), op=ALU.subtract),
        r=["c_lg", "c_mx"], w=["c_lg2"])
    act(lambda e: e.activation(out=c_lg2, in_=c_lg2, func=AF.Exp), r=["c_lg2"], w=["c_lg2"])
    dve(lambda e: e.tensor_reduce(out=c_mx, in_=c_lg2, axis=AX.X, op=ALU.add), r=["c_lg2", "c_mx"], w=["c_mx"])
    dve(lambda e: e.reciprocal(out=c_mx, in_=c_mx), r=["c_mx"], w=["c_mx"])
    dve(lambda e: e.tensor_tensor(out=c_lg2, in0=c_lg2, in1=c_mx.unsqueeze(2).to_broadcast([128, 2, 4]), op=ALU.mult),
        r=["c_lg2", "c_mx"], w=["c_lg2"])
    pool(lambda e: e.memset(lbT[:, :, 0:1], 0.0), w=["lbT"])
    for l in range(1, 4):
        dve(lambda e, l=l: e.tensor_tensor(out=lbT[:, :, l:l + 1], in0=lbT[:, :, l - 1:l], in1=c_lg2[:, :, l:l + 1], op=ALU.add),
            r=["c_lg2", "lbT"], w=["lbT"])
    dve(lambda e: e.tensor_scalar(out=omlbT[:], in0=lbT[:], scalar1=-1.0, scalar2=1.0, op0=ALU.mult, op1=ALU.add),
        r=["lbT"], w=["omlbT"])
    S.barrier()

    def load_w(slot, layer_, grp, ncols=512):
        for dc0 in (0, 4):
            S.add("pool", lambda e, dc0=dc0: e.dma_start(
                out=wbuf[slot][:, dc0:dc0 + 4, 0:ncols],
                in_=P["wpk"][layer_, grp].rearrange("p (dc f) -> p dc f", f=512)[:, dc0:dc0 + 4, 0:ncols]),
                writes=[("w", slot)], dma=f"w{slot}")

    def mm_fm(out_ap, slot, wc0, m, tok0, ntok, reads, writes, wsel=None):
        def fn(e):
            ins = None
            for dc in range(8):
                lhsT = wsel(dc) if wsel is not None else wbuf[slot][:, dc, wc0:wc0 + m]
                ins = e.matmul(out_ap, lhsT=lhsT, rhs=hT[:, dc, tok0:tok0 + ntok], start=(dc == 0), stop=(dc == 7))
            return ins
        blks = range(tok0 // 128, (tok0 + ntok) // 128)
        pe(fn, r=[("w", slot)] + [("hT", b) for b in blks] + list(reads), w=writes)

    def mm_tm(out_ap, slot, wc0, n, blk, writes):
        def fn(e):
            ins = None
            for dc in range(8):
                ins = e.matmul(out_ap, lhsT=hT[:, dc, blk * 128:(blk + 1) * 128], rhs=wbuf[slot][:, dc, wc0:wc0 + n],
                               start=(dc == 0), stop=(dc == 7))
            return ins
        pe(fn, r=[("w", slot), ("hT", blk)], w=writes)

    def rstd_cols(ss_ap, out_ap, inv_n, n, tag):
        act(lambda e: e.activation(out=out_ap, in_=ss_ap, func=AF.Ln, scale=inv_n, bias=epsc[:, 0:1]), r=[tag + "ss"], w=[tag + "rs"])
        act(lambda e: e.activation(out=out_ap, in_=out_ap, func=AF.Exp, scale=-0.5), r=[tag + "rs"], w=[tag + "rs"])

    epsc = sb("epsc", [128, 2], F32)
    pool(lambda e: e.memset(epsc[:, 0:1], EPS), w=["epsc"])
    pool(lambda e: e.memset(epsc[:, 1:2], 1.0), w=["epsc"])
    S.barrier()

    for seq in range(n_seq):
        for b in range(NB):
            S.add("sp", lambda e, b=b, seq=seq: e.dma_start(out=x_sb[:, b, :], in_=x_d[seq, b * 128:(b + 1) * 128, :]),
                  writes=[("x", b)], dma=f"xld{b}")
        for layer in range(n_layers):
            lam_init = 0.8 - 0.6 * math.exp(-0.3 * layer)
            prm = lambda fn, w: S.add("sp", fn, writes=w, dma="prm")
            prm(lambda e: e.dma_start(out=gpreT[:], in_=P["norm_pre"][layer].rearrange("(c p) -> p c", p=128),
                                      allow_slow_non_contiguous=True), ["gpreT"])
            prm(lambda e: e.dma_start(out=gpost[:], in_=P["norm_post"][layer].partition_broadcast(128)), ["gpost"])
            prm(lambda e: e.dma_start(out=nwrep[:], in_=P["diff_norm"][layer].partition_broadcast(128)), ["nwrep"])
            for i, nm in enumerate(("diff_lq1", "diff_lk1", "diff_lq2", "diff_lk2")):
                prm(lambda e, i=i, nm=nm: e.dma_start(out=lqk[:, i, :], in_=P[nm][layer].partition_broadcast(128)), ["lqk"])
            for c_, nm in ((C_NBAF, "gla_ba_fwd"), (C_NBAB, "gla_ba_bwd")):
                prm(lambda e, c_=c_, nm=nm: e.dma_start(out=col(c_), in_=P[nm][layer].rearrange("(p o) -> p o", o=1)), [("sp", c_)])
            for c_, nm in ((C_GLANW, "gla_norm"), (C_HGNW, "hgrn_norm")):
                for half in range(2):
                    prm(lambda e, c_=c_, nm=nm, half=half: e.dma_start(out=smallp[64 * half:64 * half + 64, c_:c_ + 1],
                                                                        in_=P[nm][layer].rearrange("(p o) -> p o", o=1)), [("sp", c_)])
            pool(lambda e: e.memset(waf[:], 0.0), w=["waf"])
            pool(lambda e: e.memset(wab[:], 0.0), w=["wab"])
            S.add("pool", lambda e: e.dma_start(out=waf[0:16, :], in_=P["gla_wa2_fwd"][layer]), reads=[], writes=["waf"], dma="prmc")
            S.add("pool", lambda e: e.dma_start(out=wab[16:32, :], in_=P["gla_wa2_bwd"][layer]), reads=[], writes=["wab"], dma="prmc")
            for c_ in (C_NBAF, C_NBAB):
                dve(lambda e, c_=c_: e.tensor_scalar(out=col(c_), in0=col(c_), scalar1=-1.0, scalar2=None, op0=ALU.mult),
                    r=[("sp", c_)], w=[("sp", c_)])
            dve(lambda e: e.tensor_scalar(out=nwrep[:], in0=nwrep[:], scalar1=1.0 - lam_init, scalar2=None, op0=ALU.mult),
                r=["nwrep"], w=["nwrep"])
            dve(lambda e: e.tensor_tensor(out=lqk[:, 0, :], in0=lqk[:, 0, :], in1=lqk[:, 1, :], op=ALU.mult), r=["lqk"], w=["lqk"])
            dve(lambda e: e.tensor_tensor(out=lqk[:, 2, :], in0=lqk[:, 2, :], in1=lqk[:, 3, :], op=ALU.mult), r=["lqk"], w=["lqk"])
            dve(lambda e: e.tensor_reduce(out=col(C_T0), in_=lqk[:, 0, :], axis=AX.X, op=ALU.add), r=["lqk"], w=["cT0"])
            dve(lambda e: e.tensor_reduce(out=col(C_T1), in_=lqk[:, 2, :], axis=AX.X, op=ALU.add), r=["lqk"], w=["cT1"])
            act(lambda e: e.activation(out=col(C_T0), in_=col(C_T0), func=AF.Exp), r=["cT0"], w=["cT0"])
            act(lambda e: e.activation(out=col(C_T1), in_=col(C_T1), func=AF.Exp), r=["cT1"], w=["cT1"])
            dve(lambda e: e.tensor_tensor(out=col(C_LAM), in0=col(C_T0), in1=col(C_T1), op=ALU.subtract), r=["cT0", "cT1"], w=["lam"])
            dve(lambda e: e.tensor_scalar(out=col(C_NLAM), in0=col(C_LAM), scalar1=-1.0, scalar2=-lam_init, op0=ALU.mult, op1=ALU.add),
                r=["lam"], w=["nlam"])
            S.barrier()

            A = Arena()
            nbA = NB if stop >= 1 else 0
            hn = [A.take([128, D], BF16) for _ in range(2)]
            junk = A.take([128, D], BF16)
            for b in range(nbA):
                act(lambda e, b=b: e.activation(out=junk, in_=x_sb[:, b, :], func=AF.Square, accum_out=col(C_SS + b)),
                    r=[("x", b), "junk"], w=["junk", ("ss", b)])
                act(lambda e, b=b: e.activation(out=col(C_RS + b), in_=col(C_SS + b), func=AF.Ln, scale=1.0 / D, bias=epsc[:, 0:1]),
                    r=[("ss", b)], w=[("rs", b)])
                act(lambda e, b=b: e.activation(out=col(C_RS + b), in_=col(C_RS + b), func=AF.Exp, scale=-0.5),
                    r=[("rs", b)], w=[("rs", b)])
                dve(lambda e, b=b: e.tensor_scalar(out=hn[b % 2], in0=x_sb[:, b, :], scalar1=col(C_RS + b), scalar2=None, op0=ALU.mult),
                    r=[("x", b), ("rs", b)], w=[("hn", b % 2)])
                bank = 6 + (b % 2)

                def tr(e, b=b, bank=bank):
                    ins = None
                    for dc in range(8):
                        ins = e.transpose(out=pbf(bank)[:, dc * 128:(dc + 1) * 128], in_=hn[b % 2][:, dc * 128:(dc + 1) * 128],
                                          identity=ident[:])
                    return ins
                pe(tr, r=[("hn", b % 2), "ident"], w=[("pb", bank)])

                def evacA(b_):
                    bank_ = 6 + (b_ % 2)
                    dve(lambda e: e.tensor_tensor(
                        out=hT[:, :, b_ * 128:(b_ + 1) * 128], in0=pbf(bank_).rearrange("p (c t) -> p c t", t=128),
                        in1=gpreT[:].unsqueeze(2).to_broadcast([128, 8, 128]), op=ALU.mult),
                        r=[("pb", bank_), "gpreT"], w=[("hT", b_)])
                if b >= 1:
                    evacA(b - 1)
                if b == nbA - 1:
                    evacA(b)
            S.barrier()

            for hp in range(2 if stop >= 2 else 0):
                A = Arena()
                kH = [A.take([128, L], BF16) for _ in range(2)]
                v1 = A.take([128, NB, 2 * 130], BF16)
                qH = [[A.take([128, 512], BF16) for _ in range(2)] for _ in range(2)]
                rtA = [A.take([128, 512], BF16) for _ in range(2)]
                rtB = [A.take([128, 512], BF16) for _ in range(2)]
                sg = [A.take([128, 4, 256], BF16) for _ in range(2)]
                T = [A.take([128, 512], F32) for _ in range(2)]
                On0 = A.take([128, 4, 132], F32)
                o_t = A.take([128, 4, 132], F32)
                PT = [A.take([128, 512], BF16) for _ in range(4)]
                ytok = [A.take([128, 128], BF16) for _ in range(4)]
                rcol = A.take([128, 16], F32)
                junk2 = A.take([128, 128], BF16)
                load_w(0, layer, 2 * hp)
                load_w(1, layer, 2 * hp + 1)
                pool(lambda e: e.memset(v1[:, :, :], 1.0), w=[("v1", b) for b in range(NB)])

                def wselAB(base, ab):
                    return lambda dc: wbuf[0][:, dc, base + ab * 128:base + ab * 128 + 128]

                rope_i = [0]

                def rope(psA, psB, tok0, dst, dcol0, key, wtag):
                    i = rope_i[0] % 2
                    rope_i[0] += 1
                    cs = cos_t[:, tok0:tok0 + 512]
                    sn = sin_t[:, tok0:tok0 + 512]
                    dve(lambda e: e.tensor_tensor(out=T[0], in0=pb[psA][:, :], in1=cs, op=ALU.mult), r=[("pb", psA), "T0"], w=["T0"])
                    dve(lambda e: e.tensor_tensor(out=T[1], in0=pb[psB][:, :], in1=sn, op=ALU.mult), r=[("pb", psB), "T1"], w=["T1"])
                    pool(lambda e: e.tensor_tensor(out=rtA[i], in0=T[0], in1=T[1], op=ALU.subtract), r=["T0", "T1", ("rtA", i)], w=[("rtA", i)])
                    dve(lambda e: e.tensor_tensor(out=T[0], in0=pb[psB][:, :], in1=cs, op=ALU.mult), r=[("pb", psB), "T0"], w=["T0"])
                    dve(lambda e: e.tensor_tensor(out=T[1], in0=pb[psA][:, :], in1=sn, op=ALU.mult), r=[("pb", psA), "T1"], w=["T1"])
                    pool(lambda e: e.tensor_tensor(out=rtB[i], in0=T[0], in1=T[1], op=ALU.add), r=["T0", "T1", ("rtB", i)], w=[("rtB", i)])
                    for hm in range(4):
                        hl_, c_ = hm // 2, hm % 2
                        for ab, rt, rtag in ((0, rtA, "rtA"), (1, rtB, "rtB")):
                            S.add("sp", lambda e, hm=hm, hl_=hl_, c_=c_, ab=ab, rt=rt: e.dma_start(
                                out=dst[hl_][64 * c_ + 32 * ab:64 * c_ + 32 * ab + 32, dcol0:dcol0 + 512],
                                in_=rt[i][32 * hm:32 * hm + 32, :]), reads=[(rtag, i)], writes=[wtag(hl_)], dma=key)

                for tg in range(NSEG):
                    mm_fm(pb[0][:, :], 0, 0, 128, tg * 512, 512, [], [("pb", 0)], wsel=wselAB(256, 0))
                    mm_fm(pb[1][:, :], 0, 0, 128, tg * 512, 512, [], [("pb", 1)], wsel=wselAB(256, 1))
                    rope(0, 1, tg * 512, kH, tg * 512, "kH", lambda hl_, tg=tg: ("kH", hl_, tg))
                for b in range(NB):
                    bank = 4 + (b % 2)
                    mm_tm(pb[bank][:, 0:256], 1, 0, 256, b, [("pb", bank)])
                    act(lambda e, b=b, bank=bank: e.copy(out=v1[:, b, :].rearrange("p (h c) -> p h c", c=130)[:, :, 0:128],
                                                          in_=pb[bank][:, 0:256].rearrange("p (h c) -> p h c", c=128)),
                        r=[("pb", bank)], w=[("v1", b)])
                def prep_q(qg):
                    par = qg % 2
                    mm_fm(pb[6][:, :], 0, 0, 128, qg * 512, 512, [], [("pb", 6)], wsel=wselAB(0, 0))
                    mm_fm(pb[7][:, :], 0, 0, 128, qg * 512, 512, [], [("pb", 7)], wsel=wselAB(0, 1))
                    rope(6, 7, qg * 512, qH[par], 0, "qH%d" % par, lambda hl_, par=par: ("qH", par, hl_))
                    for bl in range(4):
                        b = qg * 4 + bl
                        bank = 6 + bl % 2
                        mm_tm(pb[bank][:, 0:256], 1, 256, 256, b, [("pb", bank)])
                        act(lambda e, bl=bl, bank=bank, par=par: e.activation(out=sg[par][:, bl, :], in_=pb[bank][:, 0:256], func=AF.Silu),
                            r=[("pb", bank)], w=[("sg", par, bl)])
                        pool(lambda e, bl=bl, par=par: e.tensor_tensor(
                            out=sg[par][:, bl, :].rearrange("p (h v) -> p h v", v=128), in0=sg[par][:, bl, :].rearrange("p (h v) -> p h v", v=128),
                            in1=nwrep[:].unsqueeze(1).to_broadcast([128, 2, 128]), op=ALU.mult),
                            r=[("sg", par, bl), "nwrep"], w=[("sg", par, bl)])

                def emit_S(qg, hm, kc, sbank):
                    par = qg % 2

                    hl_, c_ = hm // 2, hm % 2

                    def smm(e):
                        return e.matmul(pb[sbank][:, :], lhsT=kH[hl_][64 * c_:64 * c_ + 64, kc * 128:(kc + 1) * 128],
                                        rhs=qH[par][hl_][64 * c_:64 * c_ + 64, :], start=True, stop=True, tile_position=(64 * c_, 0))
                    pe(smm, r=[("kH", hl_, kc // 4), ("qH", par, hl_)], w=[("pb", sbank)])

                def emit_PV(hl, kc, sbank, ptb):
                    act(lambda e: e.activation(out=PT[ptb], in_=pb[sbank][:, :], func=AF.Exp, scale=0.125),
                        r=[("pb", sbank)], w=[("PT", ptb)])

                    def pv(e):
                        ins = None
                        for qb in range(4):
                            ins = e.matmul(pb[2 + qb][:, 0:129], lhsT=PT[ptb][:, qb * 128:(qb + 1) * 128],
                                           rhs=v1[:, kc, hl * 130:hl * 130 + 129], start=(kc == 0), stop=(kc == NB - 1))
                        return ins
                    pe(pv, r=[("PT", ptb), ("v1", kc)], w=[("pb", 2 + qb) for qb in range(4)])

                def evacuate(qg, hl, c):
                    par = qg % 2
                    dst, dtag = (On0, "On0") if c == 0 else (o_t, "o_t")
                    for qb in range(4):
                        ob = 2 + qb
                        if qb % 2 == 0:
                            dve(lambda e, ob=ob, qb=qb: e.tensor_copy(out=dst[:, qb, 0:129], in_=pb[ob][:, 0:129]),
                                r=[("pb", ob)], w=[(dtag, qb)])
                        else:
                            act(lambda e, ob=ob, qb=qb: e.copy(out=dst[:, qb, 0:129], in_=pb[ob][:, 0:129]),
                                r=[("pb", ob)], w=[(dtag, qb)])
                    if c == 0:
                        return None
                    head = 2 * hp + hl
                    tb = 6 + hl

                    def st0():
                        for qb in range(4):
                            dve(lambda e, qb=qb: e.reciprocal(out=rcol[:, qb:qb + 1], in_=On0[:, qb, 128:129]), r=[("On0", qb)], w=[("rc", qb)])
                            dve(lambda e, qb=qb: e.reciprocal(out=rcol[:, 8 + qb:9 + qb], in_=o_t[:, qb, 128:129]), r=[("o_t", qb)], w=[("rc1", qb)])
                            dve(lambda e, qb=qb: e.tensor_tensor(out=rcol[:, 8 + qb:9 + qb], in0=rcol[:, 8 + qb:9 + qb], in1=col(C_NLAM),
                                                                 op=ALU.mult), r=[("rc1", qb), "nlam"], w=[("rc1", qb)])
                            dve(lambda e, qb=qb: e.tensor_scalar(out=On0[:, qb, 0:128], in0=On0[:, qb, 0:128], scalar1=rcol[:, qb:qb + 1],
                                                                 scalar2=None, op0=ALU.mult), r=[("On0", qb), ("rc", qb)], w=[("On0", qb)])
                            dve(lambda e, qb=qb: e.scalar_tensor_tensor(out=o_t[:, qb, 0:128], in0=o_t[:, qb, 0:128],
                                                                        scalar=rcol[:, 8 + qb:9 + qb], in1=On0[:, qb, 0:128],
                                                                        op0=ALU.mult, op1=ALU.add),
                                r=[("o_t", qb), ("rc1", qb), ("On0", qb)], w=[("o_t", qb)])
                            dve(lambda e, qb=qb: e.scalar_tensor_tensor(out=junk2, in0=o_t[:, qb, 0:128], scalar=1.0, in1=o_t[:, qb, 0:128],
                                                                        op0=ALU.mult, op1=ALU.mult, accum_out=rcol[:, 4 + qb:5 + qb]),
                                r=[("o_t", qb), "junk2"], w=["junk2", ("rss", qb)])

                    def st1():
                        act(lambda e: e.activation(out=rcol[:, 4:8], in_=rcol[:, 4:8], func=AF.Ln, scale=1.0 / 128, bias=epsc[:, 0:1]),
                            r=[("rss", q_) for q_ in range(4)], w=[("rss", q_) for q_ in range(4)])
                        act(lambda e: e.activation(out=rcol[:, 4:8], in_=rcol[:, 4:8], func=AF.Exp, scale=-0.5),
                            r=[("rss", q_) for q_ in range(4)], w=[("rss", q_) for q_ in range(4)])

                    def st2():
                        for qb in range(4):
                            dve(lambda e, qb=qb: e.scalar_tensor_tensor(out=ytok[qb], in0=o_t[:, qb, 0:128], scalar=rcol[:, 4 + qb:5 + qb],
                                                                        in1=sg[par][:, qb, hl * 128:(hl + 1) * 128],
                                                                        op0=ALU.mult, op1=ALU.mult),
                                r=[("o_t", qb), ("rss", qb), ("sg", par, qb), ("ytok", qb)], w=[("ytok", qb)])

                    def st3():
                        def tr4(e):
                            ins = None
                            for qb in range(4):
                                ins = e.transpose(out=pbf(tb)[:, qb * 128:(qb + 1) * 128], in_=ytok[qb], identity=ident[:])
                            return ins
                        pe(tr4, r=[("ytok", q_) for q_ in range(4)], w=[("pb", tb)])

                    def st4():
                        dve(lambda e: e.tensor_copy(out=yT[:, 2 + head, qg * 512:(qg + 1) * 512], in_=pbf(tb)[:, 0:512]),
                            r=[("pb", tb)], w=[("yT", 2 + head, qg * 4 + q_) for q_ in range(4)])
                    return [(2, st0), (5, st1), (8, st2), (11, st3), (14, st4)]

                its = [(qg, hl, c, kc) for qg in range(NSEG) for hl in range(2) for c in range(2) for kc in range(NB)]
                prep_q(0)
                deferred = []
                qg0, hl0, c0, kc0 = its[0]
                emit_S(qg0, 2 * hl0 + c0, kc0, 0)
                for i, (qg, hl, c, kc) in enumerate(its):
                    if i + 1 < len(its):
                        qg1, hl1, c1, kc1 = its[i + 1]
                        if qg1 != qg:
                            pass
                        emit_S(qg1, 2 * hl1 + c1, kc1, (i + 1) % 2)
                    emit_PV(hl, kc, i % 2, i % 4)
                    if kc == NB - 1:
                        t_ = evacuate(qg, hl, c)
                        if t_ is not None:
                            for dl, fn_ in t_:
                                deferred.append((i + dl, fn_))
                            deferred.sort(key=lambda z: z[0])
                    if (i % 64) == 24 and qg + 1 < NSEG:
                        prep_q(qg + 1)
                    while deferred and deferred[0][0] <= i:
                        deferred.pop(0)[1]()
                while deferred:
                    deferred.pop(0)[1]()
                S.barrier()

            for tile in range(DBG["tiles"] if stop >= 3 else 0):
                stg = DBG["stage"]
                is_gla = tile == 0
                NH = 4 if is_gla else 2
                dk = 32 if is_gla else 64
                npair = 2 if is_gla else 1
                sc = -1.0 / 16 if is_gla else 1.0
                qscale = 32 ** -0.5 if is_gla else 1.0
                nwc = C_GLANW if is_gla else C_HGNW
                A = Arena()
                qbT = A.take([128, L], BF16)
                kbT = A.take([128, L], BF16)
                Sb = A.take([128, 64, 64], BF16)
                vtok = A.take([128, NB, NH * 64], BF16)
                Sf = A.take([128, 17, 64], BF16)
                gcb = A.take([128, 64], F32)
                gcf = A.take([128, 64], F32)
                T = [A.take([128, 512], F32) for _ in range(4)]
                qfT = [A.take([128, 512], BF16) for _ in range(2)]
                kfT = [A.take([128, 512], BF16) for _ in range(2)]
                khT = A.take([128, 512], BF16)
                khtok = A.take([128, 4, 128], BF16)
                Am = [A.take([128, 128], BF16) for _ in range(4)]
                sq = A.take([128, 512], BF16)
                sgs = khT
                aT = sq[0:32, :]
                if is_gla:
                    load_w(0, layer, 4)
                    load_w(1, layer, 5, 288)
                    QC, KC, VC, GC, AC = 0, 128, 256, 0, 256
                else:
                    t_ = tile - 1
                    load_w(0, layer, 6 if t_ == 0 else 8)
                    if tile == 1:
                        load_w(1, layer, 7, 256)
                    QC, ZFC, ZBC, VC, GC = 0, 128, 256, 384, t_ * 128
                    lbc = lbT[:, t_, layer:layer + 1]
                    omlbc = omlbT[:, t_, layer:layer + 1]

                def chain(seg, bwd):
                    tok0 = seg * 512
                    ksrc = None
                    if is_gla:
                        mm_fm(pb[0][0:32, :], 1, AC, 32, tok0, 512, [], [("pb", 0)])
                        act(lambda e: e.copy(out=aT, in_=pb[0][0:32, :]), r=[("pb", 0), "sq"], w=["sq"])
                        wp = wab if bwd else waf
                        pe(lambda e: e.matmul(pb[1][:, :], lhsT=wp[:], rhs=aT, start=True, stop=True),
                           r=["sq", "waf", "wab"], w=[("pb", 1)])
                        nb_ = col(C_NBAB if bwd else C_NBAF)
                        act(lambda e: e.activation(out=T[1], in_=pb[1][:, :], func=AF.Exp, scale=-1.0, bias=nb_),
                            r=[("pb", 1), "T1", ("sp", C_NBAF), ("sp", C_NBAB)], w=["T1"])
                        act(lambda e: e.activation(out=T[0], in_=T[1], func=AF.Ln, bias=epsc[:, 1:2]), r=["T1", "T0"], w=["T0"])
                    else:
                        zc = ZBC if bwd else ZFC
                        mm_fm(pb[1][:, :], 0, zc, 128, tok0, 512, [], [("pb", 1)])
                        dve(lambda e: e.tensor_scalar(out=T[0], in0=pb[1][:, :], scalar1=-69.0, scalar2=None, op0=ALU.max),
                            r=[("pb", 1), "T0"], w=["T0"])
                        act(lambda e: e.activation(out=T[1], in_=T[0], func=AF.Sigmoid), r=["T0", "T1"], w=["T1"])
                        dve(lambda e: e.tensor_scalar(out=T[2], in0=T[1], scalar1=omlbc, scalar2=lbc, op0=ALU.mult, op1=ALU.add),
                            r=["T1", "T2", "lbT", "omlbT"], w=["T2"])
                        act(lambda e: e.activation(out=T[0], in_=T[2], func=AF.Ln), r=["T2", "T0"], w=["T0"])
                        pool(lambda e: e.tensor_scalar(out=T[3], in0=T[2], scalar1=-1.0, scalar2=1.0, op0=ALU.mult, op1=ALU.add),
                             r=["T2", "T3"], w=["T3"])
                        ksrc = T[3]
                    dve(lambda e: e.tensor_tensor_scan(out=T[1], data0=rmask[:], data1=T[0], initial=0.0, op0=ALU.mult, op1=ALU.add),
                        r=["T0", "T1", "rmask"], w=["T1"])
                    v3 = lambda t: t.rearrange("p (c k) -> p c k", k=32)
                    if bwd:
                        dve(lambda e: e.tensor_tensor(out=T[2], in0=T[0], in1=T[1], op=ALU.subtract), r=["T0", "T1", "T2"], w=["T2"])
                        dve(lambda e: e.tensor_tensor(out=v3(T[2]), in0=v3(T[2]), in1=v3(T[1])[:, :, 31:32].to_broadcast([128, 16, 32]),
                                                       op=ALU.add), r=["T1", "T2"], w=["T2"])
                        Bt, Btag, e_, ie_, etag, ietag = T[2], "T2", T[0], T[1], "T0", "T1"
                        gsel = v3(T[2])[:, :, 0]
                        gc = gcb
                    else:
                        Bt, Btag, e_, ie_, etag, ietag = T[1], "T1", T[0], T[2], "T0", "T2"
                        gsel = v3(T[1])[:, :, 31]
                        gc = gcf
                    act(lambda e: e.activation(out=gc[:, seg * 16:(seg + 1) * 16], in_=gsel, func=AF.Exp, scale=sc),
                        r=[Btag], w=[("gc", bwd, seg)])
                    act(lambda e: e.activation(out=e_, in_=Bt, func=AF.Exp, scale=sc), r=[Btag, etag], w=[etag])
                    act(lambda e: e.activation(out=ie_, in_=Bt, func=AF.Exp, scale=-sc), r=[Btag, ietag], w=[ietag])
                    return e_, etag, ie_, ietag, ksrc, gc

                def qk_products(seg, bwd, e_, etag, ie_, ietag, ksrc, q_out, k_out, qtag, ktag):
                    tok0 = seg * 512
                    mm_fm(pb[0][:, :], 0, QC, 128, tok0, 512, [], [("pb", 0)])
                    dve(lambda e: e.scalar_tensor_tensor(out=q_out, in0=pb[0][:, :], scalar=qscale, in1=e_, op0=ALU.mult, op1=ALU.mult),
                        r=[("pb", 0), etag] + qtag, w=qtag)
                    if is_gla:
                        mm_fm(pb[1][:, :], 0, KC, 128, tok0, 512, [], [("pb", 1)])
                        dve(lambda e: e.tensor_tensor(out=k_out, in0=pb[1][:, :], in1=ie_, op=ALU.mult),
                            r=[("pb", 1), ietag] + ktag, w=ktag)
                    else:
                        pool(lambda e: e.tensor_tensor(out=k_out, in0=ksrc, in1=ie_, op=ALU.mult), r=["T3", ietag] + ktag, w=ktag)

                UB = (2, 3, 7, 4)
                OB = (5, 6)

                def khat_and_U(seg, bwd, k_in, ktag, gc):
                    v3 = lambda t: t.rearrange("p (c k) -> p c k", k=32)
                    pool(lambda e: e.tensor_tensor(out=v3(khT), in0=v3(k_in),
                                                   in1=gc[:, seg * 16:(seg + 1) * 16].unsqueeze(2).to_broadcast([128, 16, 32]), op=ALU.mult),
                         r=ktag + [("gc", bwd, seg), "khT"], w=["khT"])

                    def tr(e):
                        ins = None
                        for bl in range(4):
                            ins = e.transpose(out=pbf(6)[:, bl * 128:(bl + 1) * 128], in_=khT[:, bl * 128:(bl + 1) * 128], identity=ident[:])
                        return ins
                    pe(tr, r=["khT"], w=[("pb", 6)])
                    act(lambda e: e.copy(out=khtok.rearrange("p a b -> p (a b)"), in_=pbf(6)[:, 0:512]), r=[("pb", 6), "khtok"], w=["khtok"])

                    def umm(e):
                        ins = None
                        for cl in range(16):
                            bl, j = cl // 4, cl % 4
                            b = seg * 4 + bl
                            for h in range(NH):
                                ins = e.matmul(pb[UB[j]][h * dk:(h + 1) * dk, bl * 64:bl * 64 + 64],
                                               lhsT=khtok[32 * j:32 * j + 32, bl, h * dk:(h + 1) * dk],
                                               rhs=vtok[32 * j:32 * j + 32, b, h * 64:(h + 1) * 64],
                                               start=True, stop=True, tile_position=(32 * j, h * dk))
                        return ins
                    pe(umm, r=["khtok"] + [("vtok", seg * 4 + bl) for bl in range(4)],
                       w=[("pb", u) for u in UB] + [("pbs", u, s_) for u in UB for s_ in range(2)])

                def stageA_b(seg_):
                    e_, etag, ie_, ietag, ksrc, gc = chain(seg_, True)
                    qk_products(seg_, True, e_, etag, ie_, ietag, ksrc, qbT[:, seg_ * 512:(seg_ + 1) * 512],
                                kbT[:, seg_ * 512:(seg_ + 1) * 512], [("qbT", seg_)], [("kbT", seg_)])

                def stageA_f(seg_):
                    par_ = seg_ % 2
                    e_, etag, ie_, ietag, ksrc, gc = chain(seg_, False)
                    qk_products(seg_, False, e_, etag, ie_, ietag, ksrc, qfT[par_], kfT[par_], [("qfT", par_)], [("kfT", par_)])

                pool(lambda e: e.memset(Sb[:, 63, :], 0.0), w=[("Sb", 63)])
                stageA_b(NSEG - 1)
                for seg in range(NSEG - 1, -1, -1):
                    if seg > 0:
                        stageA_b(seg - 1)
                    for bl in range(4):
                        b = seg * 4 + bl
                        bank = 5 + (bl % 2)
                        mm_tm(pb[bank][:, 0:NH * 64], 0, VC, NH * 64, b, [("pb", bank)])
                        act(lambda e, b=b, bank=bank: e.copy(out=vtok[:, b, :], in_=pb[bank][:, 0:NH * 64]), r=[("pb", bank)], w=[("vtok", b)])
                    khat_and_U(seg, True, kbT[:, seg * 512:(seg + 1) * 512], [("kbT", seg)], gcb)
                    for cl in range(15, -1, -1):
                        c = seg * 16 + cl
                        if c == 0:
                            continue
                        dve(lambda e, c=c, cl=cl: e.scalar_tensor_tensor(
                            out=Sb[:, c - 1, :], in0=Sb[:, c, :], scalar=gcb[:, c:c + 1],
                            in1=pb[UB[cl % 4]][:, (cl // 4) * 64:(cl // 4) * 64 + 64], op0=ALU.mult, op1=ALU.add),
                            r=[("Sb", c), ("gc", True, seg), ("pb", UB[cl % 4])], w=[("Sb", c - 1)])
                pool(lambda e: e.memset(Sf[:, 0, :], 0.0), w=[("Sf", 0)])
                stageA_f(0)
                for seg in range(NSEG if stg >= 5 else 0):
                    par = seg % 2
                    khat_and_U(seg, False, kfT[par], [("kfT", par)], gcf)
                    if seg > 0:
                        dve(lambda e: e.tensor_copy(out=Sf[:, 0, :], in_=Sf[:, 16, :]), r=[("Sf", 16)], w=[("Sf", 0)])
                    for cl in range(16):
                        c = seg * 16 + cl
                        dve(lambda e, c=c, cl=cl: e.scalar_tensor_tensor(
                            out=Sf[:, cl + 1, :], in0=Sf[:, cl, :], scalar=gcf[:, c:c + 1],
                            in1=pb[UB[cl % 4]][:, (cl // 4) * 64:(cl // 4) * 64 + 64], op0=ALU.mult, op1=ALU.add),
                            r=[("Sf", cl), ("gc", False, seg), ("pb", UB[cl % 4])], w=[("Sf", cl + 1)])
                    if seg + 1 < NSEG:
                        stageA_f(seg + 1)
                    def emit_AT(pr, bl, hl):
                        b = seg * 4 + bl
                        h = 2 * pr + hl
                        hb = h * dk
                        abank = UB[hb // 32]
                        ops_ = []
                        for d_ in range(2):
                            if d_ == 0:
                                kk, qq, ktg, qtg = kfT[par][hb:hb + dk, bl * 128:(bl + 1) * 128], \
                                    qfT[par][hb:hb + dk, bl * 128:(bl + 1) * 128], ("kfT", par), ("qfT", par)
                            else:
                                kk, qq, ktg, qtg = kbT[hb:hb + dk, b * 128:(b + 1) * 128], \
                                    qbT[hb:hb + dk, b * 128:(b + 1) * 128], ("kbT", seg), ("qbT", seg)
                            pe(lambda e, kk=kk, qq=qq, d_=d_: e.matmul(
                                pb[abank][:, d_ * 128:(d_ + 1) * 128], lhsT=kk, rhs=qq, start=True, stop=True,
                                tile_position=(hb, 0)), r=[ktg, qtg], w=[("pb", abank)])
                        for d_ in range(2):
                            slot = 2 * hl + d_
                            mk = maskF if d_ == 0 else maskB
                            dve(lambda e, slot=slot, mk=mk, d_=d_: e.tensor_tensor(
                                out=Am[slot], in0=pb[abank][:, d_ * 128:(d_ + 1) * 128], in1=mk[:], op=ALU.mult),
                                r=[("pb", abank), ("Am", slot)], w=[("Am", slot)])

                    def emit_omm(pr, bl, hl):
                        b = seg * 4 + bl
                        h = 2 * pr + hl
                        hb = h * dk
                        slots = (2 * hl, 2 * hl + 1)

                        def omm(e):
                            oc = pb[OB[hl]][64 * hl:64 * hl + 64, bl * 128:(bl + 1) * 128]
                            e.matmul(oc, lhsT=vtok[:, b, h * 64:(h + 1) * 64], rhs=Am[slots[0]], start=True, stop=False,
                                     tile_position=(0, 64 * hl))
                            e.matmul(oc, lhsT=vtok[:, b, h * 64:(h + 1) * 64], rhs=Am[slots[1]], start=False, stop=False,
                                     tile_position=(0, 64 * hl))
                            ins = None
                            for j in range(4):
                                cl = bl * 4 + j
                                e.matmul(pb[OB[hl]][64 * hl:64 * hl + 64, bl * 128 + 32 * j:bl * 128 + 32 * j + 32],
                                         lhsT=Sf[hb:hb + dk, cl, :], rhs=qfT[par][hb:hb + dk, bl * 128 + 32 * j:bl * 128 + 32 * j + 32],
                                         start=False, stop=False, tile_position=(hb, 64 * hl))
                            for j in range(4):
                                c = b * 4 + j
                                ins = e.matmul(pb[OB[hl]][64 * hl:64 * hl + 64, bl * 128 + 32 * j:bl * 128 + 32 * j + 32],
                                               lhsT=Sb[hb:hb + dk, c, :], rhs=qbT[hb:hb + dk, b * 128 + 32 * j:b * 128 + 32 * j + 32],
                                               start=False, stop=(j == 3), tile_position=(hb, 64 * hl))
                            return ins
                        pe(omm, r=[("vtok", b), ("Am", slots[0]), ("Am", slots[1]), ("qfT", par), ("qbT", seg)]
                           + [("Sf", bl * 4 + j) for j in range(4)] + [("Sb", b * 4 + j) for j in range(4)], w=[("pb", OB[hl])])

                    units = [(pr, bl, hl) for pr in range(npair if stg >= 6 else 0) for bl in range(4) for hl in range(2)]
                    if units:
                        emit_AT(*units[0])
                    for ui, (pr, bl, hl) in enumerate(units):
                        if ui + 1 < len(units):
                            emit_AT(*units[ui + 1])
                        emit_omm(pr, bl, hl)
                        if not (bl == 3 and hl == 1):
                            continue
                        ym = (0 if is_gla else 5 + tile) + pr
                        if stg < 8:
                            continue
                        for hl in range(2):
                            act(lambda e, hl=hl: e.activation(out=sq[64 * hl:64 * hl + 64, :], in_=pb[OB[hl]][64 * hl:64 * hl + 64, :],
                                                              func=AF.Square), r=[("pb", OB[hl]), "sq"], w=["sq"])
                        pe(lambda e: e.matmul(pb[0][:, :], lhsT=bones[:], rhs=sq, start=True, stop=True), r=["sq"], w=[("pb", 0)])
                        act(lambda e: e.activation(out=T[3], in_=pb[0][:, :], func=AF.Ln, bias=epsc[:, 0:1]), r=[("pb", 0), "T3"], w=["T3"])
                        act(lambda e: e.activation(out=T[3], in_=T[3], func=AF.Exp, scale=-0.5), r=["T3"], w=["T3"])
                        for hl in range(2):
                            dve(lambda e, hl=hl: e.scalar_tensor_tensor(
                                out=T[0][64 * hl:64 * hl + 64, :], in0=pb[OB[hl]][64 * hl:64 * hl + 64, :],
                                scalar=smallp[64 * hl:64 * hl + 64, nwc:nwc + 1], in1=T[3][64 * hl:64 * hl + 64, :],
                                op0=ALU.mult, op1=ALU.mult), r=[("pb", OB[hl]), "T3", "T0", ("sp", nwc)], w=["T0"])
                        mm_fm(pb[1][:, :], 1, GC + pr * 128, 128, seg * 512, 512, [], [("pb", 1)])
                        act(lambda e: e.activation(out=sgs, in_=pb[1][:, :], func=AF.Silu), r=[("pb", 1), "khT"], w=["khT"])
                        pool(lambda e, ym=ym, seg=seg: e.tensor_tensor(out=yT[:, ym, seg * 512:(seg + 1) * 512], in0=T[0], in1=sgs, op=ALU.mult),
                             r=["T0", "khT"], w=[("yT", ym, seg * 4 + i) for i in range(4)])
                S.barrier()

            A = Arena()
            T = [A.take([128, 512], F32) for _ in range(2)]
            junk3 = A.take([128, 512], BF16)
            load_w(0, layer, 9)
            load_w(1, layer, 10)
            for b in range(NB if stop >= 4 else 0):
                for half in range(2):
                    bank = 2 * (b % 2) + half

                    def omm2(e, b=b, half=half, bank=bank):
                        ins = None
                        for mc in range(8):
                            ins = e.matmul(pb[bank][:, :], lhsT=yT[:, mc, b * 128:(b + 1) * 128], rhs=wbuf[half][:, mc, :],
                                           start=(mc == 0), stop=(mc == 7))
                        return ins
                    pe(omm2, r=[("w", half)] + [("yT", m, b) for m in range(8)], w=[("pb", bank)])
                    act(lambda e, b=b, half=half, bank=bank: e.activation(out=junk3, in_=pb[bank][:, :], func=AF.Square,
                                                                          accum_out=col(C_SS + 2 * (b % 2) + half)),
                        r=[("pb", bank), "junk3"], w=["junk3", ("ss2", b % 2, half)])
                pc = C_SS + 2 * (b % 2)
                rc_ = C_RS + (b % 2)
                dve(lambda e, pc=pc, rc_=rc_: e.tensor_tensor(out=col(rc_), in0=col(pc), in1=col(pc + 1), op=ALU.add),
                    r=[("ss2", b % 2, 0), ("ss2", b % 2, 1)], w=[("rs2", b % 2)])
                act(lambda e, rc_=rc_: e.activation(out=col(rc_), in_=col(rc_), func=AF.Ln, scale=1.0 / D, bias=epsc[:, 0:1]),
                    r=[("rs2", b % 2)], w=[("rs2", b % 2)])
                act(lambda e, rc_=rc_: e.activation(out=col(rc_), in_=col(rc_), func=AF.Exp, scale=-0.5), r=[("rs2", b % 2)], w=[("rs2", b % 2)])
                for half in range(2):
                    bank = 2 * (b % 2) + half
                    dve(lambda e, half=half, bank=bank, rc_=rc_: e.scalar_tensor_tensor(
                        out=T[half], in0=pb[bank][:, :], scalar=col(rc_), in1=gpost[:, half * 512:(half + 1) * 512],
                        op0=ALU.mult, op1=ALU.mult), r=[("pb", bank), ("rs2", b % 2), "gpost", ("Tn", half)], w=[("Tn", half)])
                    pool(lambda e, b=b, half=half: e.tensor_tensor(out=x_sb[:, b, half * 512:(half + 1) * 512],
                                                                   in0=x_sb[:, b, half * 512:(half + 1) * 512], in1=T[half], op=ALU.add),
                         r=[("Tn", half), ("x", b)], w=[("x", b)])
            if dbg and seq == 0 and layer == 0:
                S.add("sp", lambda e: e.dma_start(out=dbg_y, in_=yT[:]), reads=[("yT", m, b) for m in range(8) for b in range(NB)], dma="dbg")
                S.add("sp", lambda e: e.dma_start(out=dbg_h, in_=hT[:]), reads=[("hT", b) for b in range(NB)], dma="dbg")
            S.barrier()
        for b in range(NB):
            S.add("sp", lambda e, b=b, seq=seq: e.dma_start(out=out_d[seq, b * 128:(b + 1) * 128, :], in_=x_sb[:, b, :]),
                  reads=[("x", b)], dma=f"xst{b}")
    S.emit(st)
    st.close()
    return nc


def _pack_weights(w_in, w_out):
    def cols_qk(base):
        idx = []
        for ab in range(2):
            for hm in range(4):
                s = base + hm * 64 + ab * 32
                idx.extend(range(s, s + 32))
        return idx
    groups = []
    for hp in range(2):
        groups.append(cols_qk(800 + hp * 256) + cols_qk(1312 + hp * 256))
        groups.append(list(range(1824 + hp * 256, 2080 + hp * 256)) + list(range(2336 + hp * 256, 2592 + hp * 256)))
    groups.append(list(range(0, 512)))
    groups.append(list(range(512, 800)))
    for t_ in range(2):
        g = []
        for b0 in (2848, 3104, 3360, 3616):
            g.extend(range(b0 + t_ * 128, b0 + t_ * 128 + 128))
        groups.append(g)
        if t_ == 0:
            groups.append(list(range(3872, 4128)))
    out = np.zeros((4, 11, 128, 8, 512), np.float32)
    for l in range(4):
        wl = w_in[l].reshape(8, 128, IN_W)
        for gi, g in enumerate(groups):
            out[l, gi, :, :, :len(g)] = wl[:, :, g].transpose(1, 0, 2)
        wo = w_out[l].reshape(8, 128, 1024)
        out[l, 9] = wo[:, :, 0:512].transpose(1, 0, 2)
        out[l, 10] = wo[:, :, 512:1024].transpose(1, 0, 2)
    return out.reshape(4, 11, 128, 4096)


_NC_CACHE = {}


def kernel(**inputs):
    x = np.ascontiguousarray(np.asarray(inputs["x"], dtype=np.float32))
    B = x.shape[0]
    per = B // N_CORES
    if "nc" not in _NC_CACHE:
        _NC_CACHE["nc"] = build(4, per)
    nc = _NC_CACHE["nc"]
    params = {name: np.ascontiguousarray(np.asarray(inputs[name], dtype=np.float32)) for name, _ in PARAM_SPECS if name != "wpk"}
    params["wpk"] = _pack_weights(np.asarray(inputs["w_in"], dtype=np.float32), np.asarray(inputs["w_out"], dtype=np.float32))
    in_maps = []
    for c in range(N_CORES):
        m = {"x": x[c * per:(c + 1) * per]}
        m.update(params)
        in_maps.append(m)
    res = run_bass_kernel_spmd(nc, in_maps, core_ids=list(range(N_CORES)))
    out = np.concatenate([np.asarray(r["out"]) for r in res.results], axis=0)
    return out.astype(np.float32)
```
